# Optimizing a Trainium2 kernel written in Bass

```python
import math
import jax
import jax.numpy as jnp
from jax import lax
import numpy as np

D_MODEL = 1024
BATCH = 8
SEQ = 4096
DEPTH = 2

HEAD_DIM = 64
N_HEADS_FOX = D_MODEL // (2 * HEAD_DIM)
N_HEADS_DIL = D_MODEL // (2 * HEAD_DIM)
FOX_WIDTH = N_HEADS_FOX * HEAD_DIM
DIL_WIDTH = N_HEADS_DIL * HEAD_DIM
ATTN_MIX_WIDTH = FOX_WIDTH + DIL_WIDTH
ATTN_IN_WIDTH = 3 * FOX_WIDTH + N_HEADS_FOX + 3 * DIL_WIDTH
QUERY_BLOCK = 128
DIL_PATTERNS = ((128, 1), (512, 4), (2048, 16))
FOX_FORGET_BIAS_INIT = 2.0

GDN_HEAD_DIM = 128
N_HEADS_GDN = D_MODEL // GDN_HEAD_DIM
GDN_WIDTH = N_HEADS_GDN * GDN_HEAD_DIM
GDN_IN_WIDTH = 3 * GDN_WIDTH + 2 * N_HEADS_GDN + GDN_WIDTH
GDN_CONV = 4
GDN_CHUNK = 64

D_FF = D_MODEL * 7 // 2
N_EXPERTS = 8
TOP_K = 2

N_EVEN = (DEPTH + 1) // 2
N_ODD = DEPTH // 2
DEEPNORM_ALPHA = (2.0 * DEPTH) ** 0.25
DEEPNORM_BETA = (8.0 * DEPTH) ** -0.25
LN_EPS = 1e-5
RMS_EPS = 1e-6
NEG_INF = -1e30

kernel_name = 'fox_dilated_gdn_moe_deepnorm_hybrid'


def layer_norm(x, g, b):
    x32 = x.astype(jnp.float32)
    mu = jnp.mean(x32, -1, keepdims=True)
    var = jnp.mean(jnp.square(x32 - mu), -1, keepdims=True)
    y = (x32 - mu) * lax.rsqrt(var + LN_EPS) * g.astype(jnp.float32) + b.astype(jnp.float32)
    return y.astype(x.dtype)


def alibi_slopes(n):
    return jnp.asarray(2.0 ** (-8.0 * (np.arange(n) + 1) / n), dtype=jnp.float32)


def split_heads(t, n, dh):
    B, S, _ = t.shape
    return t.reshape(B, S, n, dh).transpose(0, 2, 1, 3)


def merge_heads(t):
    B, n, S, dh = t.shape
    return t.transpose(0, 2, 1, 3).reshape(B, S, n * dh)


def forgetting_attention(q, k, v, log_f):
    B, H, S, Dh = q.shape
    nb = S // QUERY_BLOCK
    cum = jnp.cumsum(log_f, axis=-1)
    qb = q.reshape(B, H, nb, QUERY_BLOCK, Dh).transpose(2, 0, 1, 3, 4)
    cb = cum.reshape(B, H, nb, QUERY_BLOCK).transpose(2, 0, 1, 3)
    key_pos = jnp.arange(S)
    scale = Dh ** -0.5

    def one_block(args):
        q_blk, c_blk, blk = args
        s = jnp.einsum('bhqd,bhkd->bhqk', q_blk, k, preferred_element_type=jnp.float32) * scale
        s = s + (c_blk[..., :, None] - cum[..., None, :])
        q_pos = blk * QUERY_BLOCK + jnp.arange(QUERY_BLOCK)
        s = jnp.where(key_pos[None, :] <= q_pos[:, None], s, NEG_INF)
        p = jax.nn.softmax(s, axis=-1)
        return jnp.einsum('bhqk,bhkd->bhqd', p.astype(v.dtype), v)

    out = lax.map(one_block, (qb, cb, jnp.arange(nb)))
    return out.transpose(1, 2, 0, 3, 4).reshape(B, H, S, Dh)


def dilated_window_branch(q, k, v, slopes, window, dilation):
    B, H, S, Dh = q.shape
    L = S // dilation
    nb = -(-L // QUERY_BLOCK)
    Lp = nb * QUERY_BLOCK
    span = window // dilation

    def to_sub(t):
        t = jnp.moveaxis(t.reshape(B, H, L, dilation, Dh), 3, 2)
        t = jnp.pad(t, ((0, 0), (0, 0), (0, 0), (0, Lp - L), (0, 0)))
        return t.reshape(B, H, dilation, nb, QUERY_BLOCK, Dh)

    def with_prev(t):
        prev = jnp.pad(t, ((0, 0), (0, 0), (0, 0), (1, 0), (0, 0), (0, 0)))[:, :, :, :nb]
        return jnp.concatenate([prev, t], axis=4)

    def from_sub(t):
        rest = t.shape[5:]
        t = t.reshape((B, H, dilation, Lp) + rest)[:, :, :, :L]
        return jnp.moveaxis(t, 2, 3).reshape((B, H, S) + rest)

    qs = to_sub(q)
    kw = with_prev(to_sub(k))
    vw = with_prev(to_sub(v))
    qi = jnp.arange(QUERY_BLOCK)[:, None]
    ki = jnp.arange(2 * QUERY_BLOCK)[None, :]
    delta = QUERY_BLOCK + qi - ki
    key_sub = (jnp.arange(nb)[:, None, None] - 1) * QUERY_BLOCK + ki[None]
    valid = (delta >= 0) & (delta <= span) & (key_sub >= 0)
    s = jnp.einsum('bhrnqd,bhrnkd->bhrnqk', qs, kw, preferred_element_type=jnp.float32) * (Dh ** -0.5)
    s = s - slopes[None, :, None, None, None, None] * (delta * dilation).astype(jnp.float32)
    s = jnp.where(valid, s, NEG_INF)
    m = jnp.max(s, -1)
    p = jnp.exp(s - m[..., None])
    l = jnp.sum(p, -1)
    o = jnp.einsum('bhrnqk,bhrnkd->bhrnqd', p, vw.astype(jnp.float32)) / l[..., None]
    return from_sub(o), from_sub(m), from_sub(l)


def dilated_mixture_attention(q, k, v):
    slopes = alibi_slopes(q.shape[1])
    branches = [dilated_window_branch(q, k, v, slopes, w, d) for (w, d) in DIL_PATTERNS]
    o = jnp.stack([br[0] for br in branches])
    m = jnp.stack([br[1] for br in branches])
    l = jnp.stack([br[2] for br in branches])
    wts = l * jnp.exp(m - jnp.max(m, 0))
    return jnp.sum(wts[..., None] * o, 0) / jnp.sum(wts, 0)[..., None]


def fox_dilated_mixer(x, w_in, forget_bias, w_out):
    h = x @ w_in
    sizes = (FOX_WIDTH, FOX_WIDTH, FOX_WIDTH, N_HEADS_FOX, DIL_WIDTH, DIL_WIDTH, DIL_WIDTH)
    qa, ka, va, fa, qb, kb, vb = jnp.split(h, np.cumsum(sizes)[:-1].tolist(), axis=-1)
    log_f = jax.nn.log_sigmoid(fa.astype(jnp.float32) + forget_bias.astype(jnp.float32)).transpose(0, 2, 1)
    oa = forgetting_attention(split_heads(qa, N_HEADS_FOX, HEAD_DIM), split_heads(ka, N_HEADS_FOX, HEAD_DIM),
                              split_heads(va, N_HEADS_FOX, HEAD_DIM), log_f)
    ob = dilated_mixture_attention(split_heads(qb, N_HEADS_DIL, HEAD_DIM), split_heads(kb, N_HEADS_DIL, HEAD_DIM),
                                   split_heads(vb, N_HEADS_DIL, HEAD_DIM))
    o = jnp.concatenate([oa.astype(x.dtype), ob.astype(x.dtype)], axis=1)
    return merge_heads(o) @ w_out


def causal_depthwise_conv(t, w):
    C = t.shape[-1]
    return lax.conv_general_dilated(t.astype(jnp.float32), w.astype(jnp.float32)[:, None, :],
                                    window_strides=(1,), padding=((GDN_CONV - 1, 0),),
                                    dimension_numbers=('NWC', 'WIO', 'NWC'), feature_group_count=C)


def l2_normalize(t):
    return t * lax.rsqrt(jnp.sum(jnp.square(t), -1, keepdims=True) + RMS_EPS)


def chunk_gated_delta_rule(q, k, v, g, beta):
    B, H, S, Dk = q.shape
    Dv = v.shape[-1]
    C = GDN_CHUNK
    N = S // C
    f32 = jnp.float32
    q, k, v, g, beta = (t.astype(f32).reshape((B, H, N, C) + t.shape[3:]) for t in (q, k, v, g, beta))
    gam = jnp.cumsum(g, -1)
    incl = jnp.tril(jnp.ones((C, C), bool))
    strict = jnp.tril(jnp.ones((C, C), bool), -1)
    diff = gam[..., :, None] - gam[..., None, :]
    decay = jnp.where(incl, jnp.exp(jnp.where(incl, diff, 0.0)), 0.0)
    a_strict = jnp.where(strict, beta[..., :, None] * jnp.einsum('bhncd,bhnjd->bhncj', k, k) * decay, 0.0)
    rhs = jnp.concatenate([v * beta[..., None], k * (beta * jnp.exp(gam))[..., None]], -1)
    sol = lax.linalg.triangular_solve(a_strict + jnp.eye(C, dtype=f32), rhs, left_side=True,
                                      lower=True, unit_diagonal=True)
    u, w = sol[..., :Dv], sol[..., Dv:]
    qk = jnp.einsum('bhncd,bhnjd->bhncj', q, k) * decay
    q_dec = q * jnp.exp(gam)[..., None]
    k_dec = k * jnp.exp(gam[..., -1:] - gam)[..., None]
    g_tot = jnp.exp(gam[..., -1])

    def step(state, inp):
        u_c, w_c, qk_c, qd_c, kd_c, gt_c = inp
        v_new = u_c - jnp.einsum('bhck,bhkv->bhcv', w_c, state)
        o_c = jnp.einsum('bhck,bhkv->bhcv', qd_c, state) + jnp.einsum('bhcj,bhjv->bhcv', qk_c, v_new)
        state = state * gt_c[..., None, None] + jnp.einsum('bhck,bhcv->bhkv', kd_c, v_new)
        return state, o_c

    xs = tuple(jnp.moveaxis(t, 2, 0) for t in (u, w, qk, q_dec, k_dec, g_tot))
    _, o = lax.scan(step, jnp.zeros((B, H, Dk, Dv), f32), xs)
    return jnp.moveaxis(o, 0, 2).reshape(B, H, S, Dv)


def gated_deltanet_mixer(x, w_in, conv_w, a_log, dt_bias, norm_g, w_out):
    f32 = jnp.float32
    h = x @ w_in
    sizes = (3 * GDN_WIDTH, N_HEADS_GDN, N_HEADS_GDN, GDN_WIDTH)
    qkv, b_logit, a_logit, gate = jnp.split(h, np.cumsum(sizes)[:-1].tolist(), axis=-1)
    qkv = jax.nn.silu(causal_depthwise_conv(qkv, conv_w))
    q, k, v = jnp.split(qkv, 3, axis=-1)
    q = l2_normalize(split_heads(q, N_HEADS_GDN, GDN_HEAD_DIM)) * (GDN_HEAD_DIM ** -0.5)
    k = l2_normalize(split_heads(k, N_HEADS_GDN, GDN_HEAD_DIM))
    v = split_heads(v, N_HEADS_GDN, GDN_HEAD_DIM)
    beta = jax.nn.sigmoid(b_logit.astype(f32)).transpose(0, 2, 1)
    g = (-jnp.exp(a_log.astype(f32)) * jax.nn.softplus(a_logit.astype(f32) + dt_bias.astype(f32))).transpose(0, 2, 1)
    o = chunk_gated_delta_rule(q, k, v, g, beta)
    o = o * lax.rsqrt(jnp.mean(jnp.square(o), -1, keepdims=True) + RMS_EPS) * norm_g.astype(f32)
    o = o * jax.nn.silu(split_heads(gate.astype(f32), N_HEADS_GDN, GDN_HEAD_DIM))
    return merge_heads(o.astype(x.dtype)) @ w_out


def swiglu(x, w_gate, w_up, w_down):
    return (jax.nn.silu(x @ w_gate) * (x @ w_up)) @ w_down


def moe_swiglu(x, router, w_gate, w_up, w_down):
    logits = (x @ router).astype(jnp.float32)
    top_v, top_i = lax.top_k(logits, TOP_K)
    top_w = jax.nn.softmax(top_v, axis=-1)
    gate = jnp.sum(jax.nn.one_hot(top_i, N_EXPERTS, dtype=jnp.float32) * top_w[..., None], axis=-2)
    y = jnp.zeros(x.shape, jnp.float32)
    for e in range(N_EXPERTS):
        y = y + gate[..., e:e + 1] * swiglu(x, w_gate[e], w_up[e], w_down[e]).astype(jnp.float32)
    return y.astype(x.dtype)


def setup_inputs(seed: int = 0) -> dict:
    key = jax.random.key(seed)
    ks = jax.random.split(key, 32)
    f32 = jnp.float32

    def nrm(k, shape, fan_in, scale=1.0):
        return jax.random.normal(k, shape, f32) * (scale * fan_in ** -0.5)

    def gain(k, shape):
        return 1.0 + 0.02 * jax.random.normal(k, shape, f32)

    def bias(k, shape):
        return 0.02 * jax.random.normal(k, shape, f32)

    E, O, D = N_EVEN, N_ODD, D_MODEL
    dt = jnp.exp(jax.random.uniform(ks[14], (O, N_HEADS_GDN), f32, math.log(1e-3), math.log(1e-1)))
    return {
        'x': jax.random.normal(ks[0], (BATCH, SEQ, D), f32),
        'attn_w_in': nrm(ks[1], (E, D, ATTN_IN_WIDTH), D),
        'fox_forget_bias': FOX_FORGET_BIAS_INIT + 0.1 * jax.random.normal(ks[2], (E, N_HEADS_FOX), f32),
        'attn_w_out': nrm(ks[3], (E, ATTN_MIX_WIDTH, D), ATTN_MIX_WIDTH, DEEPNORM_BETA),
        'ln_attn_g': gain(ks[4], (E, D)),
        'ln_attn_b': bias(ks[5], (E, D)),
        'ffn_w_gate': nrm(ks[6], (E, D, D_FF), D),
        'ffn_w_up': nrm(ks[7], (E, D, D_FF), D),
        'ffn_w_down': nrm(ks[8], (E, D_FF, D), D_FF, DEEPNORM_BETA),
        'ln_ffn_g': gain(ks[9], (E, D)),
        'ln_ffn_b': bias(ks[10], (E, D)),
        'gdn_w_in': nrm(ks[11], (O, D, GDN_IN_WIDTH), D),
        'gdn_conv_w': nrm(ks[12], (O, GDN_CONV, 3 * GDN_WIDTH), GDN_CONV),
        'gdn_a_log': jnp.log(jax.random.uniform(ks[13], (O, N_HEADS_GDN), f32, 1.0, 16.0)),
        'gdn_dt_bias': dt + jnp.log(-jnp.expm1(-dt)),
        'gdn_norm_g': gain(ks[15], (O, GDN_HEAD_DIM)),
        'gdn_w_out': nrm(ks[16], (O, GDN_WIDTH, D), GDN_WIDTH, DEEPNORM_BETA),
        'ln_gdn_g': gain(ks[17], (O, D)),
        'ln_gdn_b': bias(ks[18], (O, D)),
        'moe_router': nrm(ks[19], (O, D, N_EXPERTS), D),
        'moe_w_gate': nrm(ks[20], (O, N_EXPERTS, D, D_FF), D),
        'moe_w_up': nrm(ks[21], (O, N_EXPERTS, D, D_FF), D),
        'moe_w_down': nrm(ks[22], (O, N_EXPERTS, D_FF, D), D_FF, DEEPNORM_BETA),
        'ln_moe_g': gain(ks[23], (O, D)),
        'ln_moe_b': bias(ks[24], (O, D)),
    }


def reference(x, attn_w_in, fox_forget_bias, attn_w_out, ln_attn_g, ln_attn_b,
              ffn_w_gate, ffn_w_up, ffn_w_down, ln_ffn_g, ln_ffn_b,
              gdn_w_in, gdn_conv_w, gdn_a_log, gdn_dt_bias, gdn_norm_g, gdn_w_out, ln_gdn_g, ln_gdn_b,
              moe_router, moe_w_gate, moe_w_up, moe_w_down, ln_moe_g, ln_moe_b):
    for layer in range(DEPTH):
        i = layer // 2
        if layer % 2 == 0:
            mix = fox_dilated_mixer(x, attn_w_in[i], fox_forget_bias[i], attn_w_out[i])
            x = layer_norm(DEEPNORM_ALPHA * x + mix, ln_attn_g[i], ln_attn_b[i])
            ffn = swiglu(x, ffn_w_gate[i], ffn_w_up[i], ffn_w_down[i])
            x = layer_norm(DEEPNORM_ALPHA * x + ffn, ln_ffn_g[i], ln_ffn_b[i])
        else:
            mix = gated_deltanet_mixer(x, gdn_w_in[i], gdn_conv_w[i], gdn_a_log[i], gdn_dt_bias[i],
                                       gdn_norm_g[i], gdn_w_out[i])
            x = layer_norm(DEEPNORM_ALPHA * x + mix, ln_gdn_g[i], ln_gdn_b[i])
            ffn = moe_swiglu(x, moe_router[i], moe_w_gate[i], moe_w_up[i], moe_w_down[i])
            x = layer_norm(DEEPNORM_ALPHA * x + ffn, ln_moe_g[i], ln_moe_b[i])
    return x
```

```python
import contextlib
import math
import numpy as np
import concourse.bass as bass
import concourse.mybir as mybir
from concourse.bass_utils import run_bass_kernel_spmd

F32 = mybir.dt.float32
BF16 = mybir.dt.bfloat16
AF = mybir.ActivationFunctionType
ALU = mybir.AluOpType

SEQ = 4096
D = 1024
NT = SEQ // 128
KC = D // 128
DFF = 3584
NFC = DFF // 128
NE = 8
ALPHA = 4.0 ** 0.25
LN_EPS = 1e-5
RMS_EPS = 1e-6
NEG = -30000.0
NDEL = 17


class Sched:
    def __init__(self, nc, n_dma_sems=24):
        self.nc = nc
        self.base = contextlib.ExitStack()
        self.stacks = [self.base]
        self.eng = {"pe": nc.tensor, "dve": nc.vector, "act": nc.scalar,
                    "pool": nc.gpsimd, "sp": nc.sync}
        self.sem, self.cnt = {}, {}
        for e in self.eng:
            self.sem[e] = self.base.enter_context(nc.semaphore("s_" + e))
            self.cnt[e] = 0
        self.dsem = [self.base.enter_context(nc.semaphore(f"d{i}")) for i in range(n_dma_sems)]
        self.dcnt = [0] * n_dma_sems
        self.dnext = 0
        self.dnext_sw = 0
        self.n_hw = n_dma_sems - 8
        self.seen = {e: {} for e in self.eng}
        self.lastw, self.reads = {}, {}
        self.n_inst = 0
        self._uid = 0

    def sbuf(self, name, shape, dt):
        self._uid += 1
        return self.stacks[-1].enter_context(self.nc.sbuf_tensor(f"{name}_{self._uid}", list(shape), dt))

    def psum(self, name, shape, dt=F32):
        self._uid += 1
        return self.stacks[-1].enter_context(self.nc.psum_tensor(f"{name}_{self._uid}", list(shape), dt))

    @contextlib.contextmanager
    def scope(self):
        st = contextlib.ExitStack()
        self.stacks.append(st)
        try:
            yield
        finally:
            self.barrier()
            self.stacks.pop()
            st.close()

    def _wait(self, e, ev):
        key, val, src = ev
        if src == e and e == "pe":
            return
        if self.seen[e].get(key, 0) >= val:
            return
        semh = self.sem[key] if isinstance(key, str) else self.dsem[key]
        self.eng[e].wait_ge(semh, val)
        self.seen[e][key] = val

    def _deps(self, e, reads, writes):
        for r in reads:
            ev = self.lastw.get(r)
            if ev is not None:
                self._wait(e, ev)
        for w in writes:
            ev = self.lastw.get(w)
            if ev is not None:
                self._wait(e, ev)
            for ev in self.reads.get(w, ()):
                self._wait(e, ev)

    def _commit(self, ev, reads, writes):
        for r in reads:
            self.reads.setdefault(r, []).append(ev)
        for w in writes:
            self.lastw[w] = ev
            self.reads[w] = []

    def op(self, e, reads, writes, fn, *a, **k):
        self._deps(e, reads, writes)
        inst = fn(*a, **k)
        self.cnt[e] += 1
        inst.then_inc(self.sem[e], 1)
        ev = (e, self.cnt[e], e)
        self._commit(ev, reads, writes)
        self.n_inst += 1
        return ev

    def dma(self, reads, writes, out, in_, q="sp", **k):
        if q == "pool":
            i = self.n_hw + self.dnext_sw
            self.dnext_sw = (self.dnext_sw + 1) % (len(self.dsem) - self.n_hw)
        else:
            i = self.dnext
            self.dnext = (self.dnext + 1) % self.n_hw
        if self.dcnt[i] > 0:
            self._wait(q, (i, self.dcnt[i], "dma"))
        self._deps(q, reads, writes)
        inst = self.eng[q].dma_start(out=out, in_=in_, **k)
        self.dcnt[i] += 16
        inst.then_inc(self.dsem[i], 16)
        ev = (i, self.dcnt[i], "dma")
        self._commit(ev, reads, writes)
        self.n_inst += 1
        return ev

    def barrier(self):
        evs = [(e, self.cnt[e], e) for e in self.eng if self.cnt[e] > 0]
        evs += [(i, self.dcnt[i], "dma") for i in range(len(self.dsem)) if self.dcnt[i] > 0]
        for e in self.eng:
            for ev in evs:
                if ev[2] != e:
                    self._wait(e, ev)
        self.lastw.clear()
        self.reads.clear()

    def close(self):
        self.base.close()


def make_consts():
    c = {}
    c["ident"] = np.eye(128, dtype=np.float32)
    k = np.arange(128)[:, None]
    q = np.arange(128)[None, :]
    c["tri_u"] = (k <= q).astype(np.float32)
    c["ones"] = np.ones((128, 128), np.float32)
    c["fox_mask"] = np.where(k <= q, 0.0, NEG).astype(np.float32)
    slopes = 2.0 ** (-8.0 * (np.arange(8) + 1) / 8)
    B = np.zeros((128, 8, NDEL, 128), np.float32)
    for dl in range(NDEL):
        dist = 128 * dl + q - k
        cnt = ((dist >= 0) & (dist <= 128)).astype(np.float64)
        cnt += ((dist >= 0) & (dist % 4 == 0) & (dist <= 512))
        cnt += ((dist >= 0) & (dist % 16 == 0) & (dist <= 2048))
        for h in range(8):
            with np.errstate(divide="ignore"):
                b = np.where(cnt > 0, np.log(np.maximum(cnt, 1)) - slopes[h] * dist, NEG)
            B[:, h, dl, :] = np.maximum(b, NEG)
    c["dil_bias"] = B
    cc = np.arange(128)[:, None]
    jj = np.arange(128)[None, :]
    c["gdn_m1"] = np.where(jj > cc, 0.0, NEG).astype(np.float32)
    c["gdn_m2"] = np.where(cc >= jj, 0.0, NEG).astype(np.float32)
    return c


CONST_SHAPES = {"ident": [128, 128], "tri_u": [128, 128], "ones": [128, 128],
                "fox_mask": [128, 128], "dil_bias": [128, 8, NDEL, 128],
                "gdn_m1": [128, 128], "gdn_m2": [128, 128]}

IN_SHAPES = {
    "x": [SEQ, D], "attn_w_in": [D, 3080], "fox_forget_bias": [8], "attn_w_out": [D, D],
    "ln_attn_g": [D], "ln_attn_b": [D], "ffn_w_gate": [D, DFF], "ffn_w_up": [D, DFF],
    "ffn_w_down": [DFF, D], "ln_ffn_g": [D], "ln_ffn_b": [D],
    "gdn_w_in": [D, 4112], "gdn_conv_w": [4, 3072], "gdn_a_log": [8], "gdn_dt_bias": [8],
    "gdn_norm_g": [128], "gdn_w_out": [D, D], "ln_gdn_g": [D], "ln_gdn_b": [D],
    "moe_router": [D, NE], "moe_w_gate": [NE, D, DFF], "moe_w_up": [NE, D, DFF],
    "moe_w_down": [NE, DFF, D], "ln_moe_g": [D], "ln_moe_b": [D],
}


class Ctx:
    pass


def emit_xT(C, t, src):
    S, nc = C.S, C.nc
    b = C.xt_i % 2
    C.xt_i += 1
    xb = C.xbf[b]
    S.op("act", [src[1]], [("xbf", b)], nc.scalar.copy, xb[:], src[0])
    for g in range(2):
        pb = (2 * b + g) % 2
        pt = C.ps_tr[pb]
        for j in range(4):
            kc = g * 4 + j
            S.op("pe", [("xbf", b), "identb"], [("pstr", pb)], nc.tensor.transpose,
                 pt[:, j * 128:(j + 1) * 128], xb[:, kc * 128:(kc + 1) * 128], C.identb[:])
        S.op("dve", [("pstr", pb)], [("xT", t)], nc.vector.tensor_copy,
             C.xT[:, g * 4:(g + 1) * 4, t * 128:(t + 1) * 128],
             pt[:].rearrange("p (j c) -> p j c", j=4))


def load_bcast(C, name, vec, n):
    t = C.S.sbuf(name, [128, n], F32)
    C.S.dma([], [name], t[:], vec.partition_broadcast(128))
    return t


def ln_finish(C, t, z, zres, gB, bB, out_d, out_key, make_xT=True, router=None):
    S, nc = C.S, C.nc
    i = C.ln_i % 3
    C.ln_i += 1
    st, mv, sd = C.ln_st[i], C.ln_mv[i], C.ln_sd[i]
    for hf in range(2):
        S.op("dve", [zres], [("lnst", i)], nc.vector.bn_stats, st[:, hf, :], z[:, hf * 512:(hf + 1) * 512])
    S.op("dve", [("lnst", i)], [("lnmv", i)], nc.vector.bn_aggr, mv[:], st[:].rearrange("p a b -> p (a b)"))
    S.op("act", [("lnmv", i)], [("lnsd", i)], nc.scalar.activation, sd[:, 0:1], mv[:, 1:2], AF.Sqrt,
         bias=C.eps_ln[:, 0:1], scale=1.0)
    S.op("dve", [("lnsd", i)], [("lnrs", i)], nc.vector.reciprocal, sd[:, 1:2], sd[:, 0:1])
    zn = C.ln_zn[i]
    S.op("dve", [zres, ("lnmv", i), ("lnrs", i)], [("lnzn", i)], nc.vector.tensor_scalar,
         zn[:], z[:], mv[:, 0:1], sd[:, 1:2], ALU.subtract, ALU.mult)
    ln_flush(C)
    S.op("pool", [("lnzn", i), gB[1]], [("lnzn", i)], nc.gpsimd.tensor_tensor, zn[:], zn[:], gB[0][:], ALU.mult)
    S.op("pool", [("lnzn", i), bB[1]], [("lnzn", i)], nc.gpsimd.tensor_tensor, zn[:], zn[:], bB[0][:], ALU.add)
    S.dma([("lnzn", i)], [(out_key, t)], out_d[t * 128:(t + 1) * 128, :], zn[:], q="pool")
    def later(t=t, zn=zn, i=i):
        if make_xT:
            emit_xT(C, t, (zn[:], ("lnzn", i)))
        if router is not None:
            router(C, t, zn, ("lnzn", i))
    C.ln_pending.append(later)


def ln_flush(C):
    while C.ln_pending:
        C.ln_pending.pop(0)()


def ffn_phase1(C, wg_d, wu_d, hT_d, hkey):
    S, nc = C.S, C.nc
    for cg in range(NFC // 2):
        b = C.w1_i % 2
        C.w1_i += 1
        wg, wu = C.wg[b], C.wu[b]
        S.dma([], [("wg", b)], wg[:], wg_d[:, cg * 256:(cg + 1) * 256].rearrange("(kc p) n -> p kc n", p=128), q="pool")
        S.dma([], [("wu", b)], wu[:], wu_d[:, cg * 256:(cg + 1) * 256].rearrange("(kc p) n -> p kc n", p=128), q="pool")
        for fl in range(2):
            fc = cg * 2 + fl
            hb_i = C.hb_i % 2
            C.hb_i += 1
            hb = C.hbuf[hb_i]
            for tb in range(8):
                pi = C.p1_i % 2
                C.p1_i += 1
                pg, pu = C.ps_g[pi], C.ps_u[pi]
                for kc in range(KC):
                    S.op("pe", [("wg", b), ("xTall",)], [("psg", pi)], nc.tensor.matmul, pg[:],
                         wg[:, kc, fl * 128:(fl + 1) * 128], C.xT[:, kc, tb * 512:(tb + 1) * 512],
                         start=(kc == 0), stop=(kc == KC - 1))
                for kc in range(KC):
                    S.op("pe", [("wu", b), ("xTall",)], [("psu", pi)], nc.tensor.matmul, pu[:],
                         wu[:, kc, fl * 128:(fl + 1) * 128], C.xT[:, kc, tb * 512:(tb + 1) * 512],
                         start=(kc == 0), stop=(kc == KC - 1))
                sg = C.sg[pi]
                S.op("act", [("psg", pi)], [("sg", pi)], nc.scalar.activation, sg[:], pg[:], AF.Silu)
                S.op("dve", [("sg", pi), ("psu", pi)], [("hbuf", hb_i)], nc.vector.tensor_tensor,
                     hb[:, tb * 512:(tb + 1) * 512], sg[:], pu[:], ALU.mult)
            S.dma([("hbuf", hb_i)], [(hkey, fc)], hT_d[:, :, fc, :].rearrange("tb p k -> p tb k"),
                  hb[:].rearrange("p (tb k) -> p tb k", k=256))


def ffn_phase2(C, wd_d, hT_d, hkey, yacc_d, gate, first):
    S, nc = C.S, C.nc
    wd = C.wd
    for q4 in range(4):
        S.dma([], [("wd", q4)], wd[:, q4 * 7:(q4 + 1) * 7, :],
              wd_d[q4 * 896:(q4 + 1) * 896, :].rearrange("(fc p) n -> p fc n", p=128), q="pool")
    for tb in range(16):
        hi = C.hk_i % 2
        C.hk_i += 1
        hk = C.hblk[hi]
        S.dma([(hkey, fc) for fc in range(NFC)], [("hblk", hi)], hk[:], hT_d[tb])
        for j in range(2):
            t = tb * 2 + j
            yi = C.y_i % 2
            C.y_i += 1
            ysb = C.ysb[yi]
            if gate is not None and not first:
                S.dma([("yacc", t)], [("ysb", yi)], ysb[:], yacc_d[t * 128:(t + 1) * 128, :], q="act")
            for hf in range(2):
                pi = C.p2_i % 2
                C.p2_i += 1
                py = C.ps_y[pi]
                for fc in range(NFC):
                    S.op("pe", [("hblk", hi), ("wd", fc // 7)], [("psy", pi)], nc.tensor.matmul, py[:],
                         hk[:, fc, j * 128:(j + 1) * 128], wd[:, fc, hf * 512:(hf + 1) * 512],
                         start=(fc == 0), stop=(fc == NFC - 1))
                ys = ysb[:, hf * 512:(hf + 1) * 512]
                if gate is None:
                    S.op("act", [("psy", pi)], [("ysb", yi)], nc.scalar.copy, ys, py[:])
                elif first:
                    S.op("act", [("psy", pi), "gate"], [("ysb", yi)], nc.scalar.activation, ys, py[:],
                         AF.Copy, scale=gate[0][:, t, gate[1]:gate[1] + 1])
                else:
                    S.op("dve", [("psy", pi), "gate", ("ysb", yi)], [("ysb", yi)], nc.vector.scalar_tensor_tensor,
                         ys, py[:], gate[0][:, t, gate[1]:gate[1] + 1], ys, ALU.mult, ALU.add)
            S.dma([("ysb", yi)], [("yacc", t)], yacc_d[t * 128:(t + 1) * 128, :], ysb[:], q="act")


def ln_from_dram(C, xres_d, xres_key, yacc_d, g_d, b_d, out_d, out_key, make_xT=True, router=None):
    S, nc = C.S, C.nc
    gB = (load_bcast(C, "gB" + out_key, g_d, D), "gB" + out_key)
    bB = (load_bcast(C, "bB" + out_key, b_d, D), "bB" + out_key)
    for t in range(NT):
        i = C.lz_i % 2
        C.lz_i += 1
        xr, yy = C.lnx[i], C.lny[i]
        S.dma([(xres_key, t)], [("lnx", i)], xr[:], xres_d[t * 128:(t + 1) * 128, :])
        S.dma([("yacc", t)], [("lny", i)], yy[:], yacc_d[t * 128:(t + 1) * 128, :])
        S.op("dve", [("lnx", i), ("lny", i)], [("lny", i)], nc.vector.scalar_tensor_tensor,
             yy[:], xr[:], ALPHA, yy[:], ALU.mult, ALU.add)
        ln_finish(C, t, yy, ("lny", i), gB, bB, out_d, out_key, make_xT=make_xT, router=router)
    ln_flush(C)


def alloc_common(C):
    S, nc = C.S, C.nc
    C.xT = S.sbuf("xT", [128, KC, SEQ], BF16)
    C.ident = S.sbuf("ident", [128, 128], F32)
    C.identb = S.sbuf("identb", [128, 128], BF16)
    C.eps_ln = S.sbuf("epsln", [128, 2], F32)
    S.dma([], ["ident"], C.ident[:], C.cd["ident"])
    S.op("dve", ["ident"], ["identb"], nc.vector.tensor_copy, C.identb[:], C.ident[:])
    S.op("pool", [], ["epsln"], nc.gpsimd.memset, C.eps_ln[:, 0:1], LN_EPS)
    S.op("pool", ["epsln"], ["epsln"], nc.gpsimd.memset, C.eps_ln[:, 1:2], RMS_EPS)
    C.xbf = [S.sbuf("xbf", [128, D], BF16) for _ in range(2)]
    C.gate = S.sbuf("gate", [128, NT, NE], F32)
    C.ln_pending = []
    C.xt_i = C.ln_i = C.lz_i = 0
    C.w1_i = C.hb_i = C.p1_i = C.hk_i = C.y_i = C.p2_i = 0


def alloc_ln(C):
    S = C.S
    C.ln_st = [S.sbuf("lnst", [128, 2, 6], F32) for _ in range(3)]
    C.ln_mv = [S.sbuf("lnmv", [128, 2], F32) for _ in range(3)]
    C.ln_sd = [S.sbuf("lnsd", [128, 2], F32) for _ in range(3)]
    C.ln_zn = [S.sbuf("lnzn", [128, D], F32) for _ in range(3)]
    C.lnx = [S.sbuf("lnx", [128, D], F32) for _ in range(2)]
    C.lny = [S.sbuf("lny", [128, D], F32) for _ in range(2)]
    C.ps_tr = [S.psum("pstr", [128, 512], BF16) for _ in range(2)]


def stage_load_xT(C, x_d, key):
    S = C.S
    with S.scope():
        alloc_ln(C)
        for t in range(NT):
            i = t % 2
            S.dma([(key, t)], [("lnx", i)], C.lnx[i][:], x_d[t * 128:(t + 1) * 128, :])
            emit_xT(C, t, (C.lnx[i][:], ("lnx", i)))


def stage_ffn(C, experts, xres_d, xres_key, g_d, b_d, out_d, out_key, gate_sb=None, make_xT=True, router=None):
    S, nc = C.S, C.nc
    with S.scope():
        C.wg = [S.sbuf("wg", [128, KC, 256], BF16) for _ in range(2)]
        C.wu = [S.sbuf("wu", [128, KC, 256], BF16) for _ in range(2)]
        C.hbuf = [S.sbuf("hbuf", [128, SEQ], BF16) for _ in range(2)]
        C.sg = [S.sbuf("sg", [128, 512], BF16) for _ in range(2)]
        C.wd = S.sbuf("wd", [128, NFC, D], BF16)
        C.hblk = [S.sbuf("hblk", [128, NFC, 256], BF16) for _ in range(2)]
        C.ysb = [S.sbuf("ysb", [128, D], F32) for _ in range(2)]
        C.ps_g = [S.psum("psg", [128, 512]) for _ in range(2)]
        C.ps_u = [S.psum("psu", [128, 512]) for _ in range(2)]
        C.ps_y = [S.psum("psy", [128, 512]) for _ in range(2)]
        for e, (wg_d, wu_d, wd_d) in enumerate(experts):
            hT_d = C.hT_d[e % 2]
            hkey = "hT%d" % (e % 2)
            ffn_phase1(C, wg_d, wu_d, hT_d, hkey)
            ffn_phase2(C, wd_d, hT_d, hkey, C.yacc_d, None if gate_sb is None else (gate_sb, e), e == 0)
    with S.scope():
        alloc_ln(C)
        ln_from_dram(C, xres_d, xres_key, C.yacc_d, g_d, b_d, out_d, out_key, make_xT=make_xT, router=router)


def stage_outproj_ln(C, w_d, xres_d, xres_key, g_d, b_d, out_d, out_key, router=None):
    S, nc = C.S, C.nc
    with S.scope():
        alloc_ln(C)
        if router is not None:
            alloc_router(C)
        wout = S.sbuf("wout", [128, KC, D], BF16)
        S.dma([], ["wout"], wout[:], w_d.rearrange("(kc p) n -> p kc n", p=128), q="pool")
        gB = (load_bcast(C, "gB" + out_key, g_d, D), "gB" + out_key)
        bB = (load_bcast(C, "bB" + out_key, b_d, D), "bB" + out_key)
        ps_y = [S.psum("psyo", [128, 512]) for _ in range(3)]
        for t in range(NT):
            i = C.lz_i % 2
            C.lz_i += 1
            xr, yy = C.lnx[i], C.lny[i]
            S.dma([(xres_key, t)], [("lnx", i)], xr[:], xres_d[t * 128:(t + 1) * 128, :])
            for hf in range(2):
                pi = (2 * t + hf) % 3
                for kc in range(KC):
                    S.op("pe", ["wout", ("oTall",)], [("psyo", pi)], nc.tensor.matmul, ps_y[pi][:],
                         C.oT[:, kc, t * 128:(t + 1) * 128], wout[:, kc, hf * 512:(hf + 1) * 512],
                         start=(kc == 0), stop=(kc == KC - 1))
                S.op("dve", [("lnx", i), ("psyo", pi)], [("lny", i)], nc.vector.scalar_tensor_tensor,
                     yy[:, hf * 512:(hf + 1) * 512], xr[:, hf * 512:(hf + 1) * 512], ALPHA, ps_y[pi][:],
                     ALU.mult, ALU.add)
            ln_finish(C, t, yy, ("lny", i), gB, bB, out_d, out_key, router=router)
        ln_flush(C)


def stage_attn(C):
    S, nc, ind = C.S, C.nc, C.ind
    w_in = ind["attn_w_in"]
    KA = [S.sbuf("KA", [128, SEQ], BF16) for _ in range(2)]
    QA = [S.sbuf("QA", [128, SEQ], BF16) for _ in range(2)]
    for hh in range(2):
        S.op("pool", [], [("KA", hh)], nc.gpsimd.memset, KA[hh][:], 0.0)
        S.op("pool", [], [("QA", hh)], nc.gpsimd.memset, QA[hh][:], 0.0)
    BR = [slice(64, 70), slice(0, 6)]
    FR = [slice(0, 64), slice(64, 128)]
    vsb = S.sbuf("vsb", [128, NT, 2, 65], BF16)
    wq = S.sbuf("wq", [128, KC, 128], BF16)
    wk = S.sbuf("wk", [128, KC, 128], BF16)
    wv = S.sbuf("wv", [128, KC, 128], BF16)
    wf = S.sbuf("wf", [128, KC, 8], BF16)
    maskb = S.sbuf("maskb", [128, 128], BF16)
    dilB = S.sbuf("dilB", [128, 2, NDEL, 128], BF16)
    fbB = load_bcast(C, "fbB", ind["fox_forget_bias"], 8)
    onecol = S.sbuf("onecol", [128, 1], F32)
    triu = S.sbuf("triu", [128, 128], F32)
    ones = S.sbuf("ones", [128, 128], F32)
    lz = S.sbuf("lz", [128, NT, 8], F32)
    Lc = S.sbuf("Lc", [128, NT, 8], F32)
    tot = S.sbuf("tot", [128, NT, 8], F32)
    pend = S.sbuf("pend", [128, NT, 8], F32)
    pT = [S.sbuf("pT", [128, 512], BF16) for _ in range(3)]
    obf = [S.sbuf("obf", [128, 128], BF16) for _ in range(8)]
    pTall = S.sbuf("pTall", [128, NT, 512], BF16)

    rden = [S.sbuf("rden", [128, 1], F32) for _ in range(2)]
    ps_s = [S.psum("pss", [128, 512]) for _ in range(2)]
    ps_o = [S.psum("pso", [128, 512]) for _ in range(2)]
    ps_d = S.psum("psd", [128, 512])
    selb = S.sbuf("selb", [65, 64], F32)
    S.op("pool", [], ["selb"], nc.gpsimd.memset, selb[:], 0.0)
    S.op("pool", ["selb"], ["selb"], nc.gpsimd.memset, selb[64:65, :], 1.0)
    osb = [S.sbuf("osb", [65, 512], F32) for _ in range(2)]
    rdn = [S.sbuf("rdn", [64, 512], F32) for _ in range(2)]
    obT = [S.sbuf("obT", [64, 512], BF16) for _ in range(2)]

    def finalize(po, po_i, u, hh, G):
        S.op("act", [("pso", po_i)], [("osb", po_i)], nc.scalar.copy, osb[po_i][:], po[0:65, :])
        S.op("pe", [("osb", po_i), "selb"], ["psd"], nc.tensor.matmul, ps_d[0:64, :], selb[:], osb[po_i][:], start=True, stop=True)
        S.op("dve", ["psd"], [("rdn", po_i)], nc.vector.reciprocal, rdn[po_i][:], ps_d[0:64, :])
        S.op("dve", [("osb", po_i), ("rdn", po_i)], [("obT", po_i)], nc.vector.tensor_tensor, obT[po_i][:], osb[po_i][0:64, :], rdn[po_i][:], ALU.mult)
        r0 = u * 128 + hh * 64
        S.dma([("obT", po_i)], [("oT_d", u)], C.oT_d[r0:r0 + 64, G * 512:(G + 1) * 512], obT[po_i][:])

    ps_p = [S.psum("psp", [128, 512]) for _ in range(2)]

    S.dma([], ["maskb"], maskb[:], C.cd["fox_mask"], q="pool")
    S.dma([], ["triu"], triu[:], C.cd["tri_u"])
    S.dma([], ["ones"], ones[:], C.cd["ones"])
    S.op("pool", [], ["onecol"], nc.gpsimd.memset, onecol[:], 1.0)
    S.op("pool", [], ["vsb"], nc.gpsimd.memset, vsb[:, :, :, 64:65], 1.0)
    S.dma([], ["wf"], wf[:], w_in[:, 1536:1544].rearrange("(kc p) n -> p kc n", p=128), q="pool")

    for t in range(NT):
        pi = t % 2
        for kc in range(KC):
            S.op("pe", ["wf"], [("psp", pi)], nc.tensor.matmul, ps_p[pi][:, 0:8],
                 C.xT[:, kc, t * 128:(t + 1) * 128], wf[:, kc, :], start=(kc == 0), stop=(kc == KC - 1))
        S.op("dve", [("psp", pi), "fbB"], ["lz"], nc.vector.tensor_tensor, lz[:, t, :], ps_p[pi][:, 0:8], fbB[:], ALU.add)
    lzf = lz[:].rearrange("p a b -> p (a b)")
    S.op("act", ["lz"], ["lz"], nc.scalar.activation, lzf, lzf, AF.Exp, scale=-1.0)
    S.op("act", ["lz", "onecol"], ["lz"], nc.scalar.activation, lzf, lzf, AF.Ln, bias=onecol[:, 0:1], scale=1.0)
    S.op("pe", ["lz", "triu"], [("psp", 0)], nc.tensor.matmul, ps_p[0][:, 0:256], triu[:], lzf, start=True, stop=True)
    S.op("pe", ["lz", "ones"], [("psp", 1)], nc.tensor.matmul, ps_p[1][:, 0:256], ones[:], lzf, start=True, stop=True)
    S.op("dve", [("psp", 1)], ["tot"], nc.vector.tensor_copy, tot[:].rearrange("p a b -> p (a b)"), ps_p[1][:, 0:256])
    S.op("dve", ["tot"], ["pend"], nc.vector.tensor_copy, pend[:, 0, :], tot[:, 0, :])
    for t in range(1, NT):
        S.op("dve", ["tot", "pend"], ["pend"], nc.vector.tensor_tensor, pend[:, t, :], pend[:, t - 1, :], tot[:, t, :], ALU.add)
    S.op("dve", ["pend", "tot"], ["tot"], nc.vector.tensor_tensor, tot[:], pend[:], tot[:], ALU.subtract)
    S.op("dve", [("psp", 0), "tot"], ["Lc"], nc.vector.tensor_tensor, Lc[:].rearrange("p a b -> p (a b)"),
         ps_p[0][:, 0:256], tot[:].rearrange("p a b -> p (a b)"), ALU.add)

    npend = S.sbuf("npend", [128, NT, 8], F32)
    S.op("dve", ["pend"], ["npend"], nc.vector.tensor_scalar, npend[:].rearrange("p a b -> p (a b)"),
         pend[:].rearrange("p a b -> p (a b)"), -1.0, None, ALU.mult)
    ones3 = S.sbuf("ones3", [3, SEQ], BF16)
    S.op("pool", [], ["ones3"], nc.gpsimd.memset, ones3[:], 1.0)
    for h in range(8):
        S.dma(["ones3"], [("lrow", h)], C.lrow_d[h, 0, 3:6], ones3[:])
        S.dma(["ones3"], [("lrow", h)], C.lrow_d[h, 1, 0:3], ones3[:])
    lsp = [S.sbuf("lsp", [32, 5, 128], F32) for _ in range(2)]
    lsb = [S.sbuf("lsb", [32, 3, 128], BF16) for _ in range(2)]
    ii = 0
    for h in range(8):
        for which, src in ((0, Lc), (1, npend)):
            i = ii % 2
            ii += 1
            pp = ps_p[i]
            S.op("pe", ["Lc", "npend", "ident"], [("psp", i)], nc.tensor.transpose, pp[0:32, 0:128], src[:, :, h], C.ident[:])
            S.op("dve", [("psp", i)], [("lsb", i)], nc.vector.tensor_copy, lsb[i][:, 0, :], pp[0:32, 0:128])
            S.op("dve", [("psp", i), ("lsb", i)], [("lsp", i)], nc.vector.tensor_tensor, lsp[i][:, 0, :], pp[0:32, 0:128], lsb[i][:, 0, :], ALU.subtract)
            S.op("dve", [("lsp", i)], [("lsb", i)], nc.vector.tensor_copy, lsb[i][:, 1, :], lsp[i][:, 0, :])
            S.op("dve", [("lsp", i), ("lsb", i)], [("lsp", i)], nc.vector.tensor_tensor, lsp[i][:, 1, :], lsp[i][:, 0, :], lsb[i][:, 1, :], ALU.subtract)
            S.op("dve", [("lsp", i)], [("lsb", i)], nc.vector.tensor_copy, lsb[i][:, 2, :], lsp[i][:, 1, :])
            S.dma([("lsb", i)], [("lrow", h)], C.lrow_d[h, which, which * 3:which * 3 + 3].rearrange("c (t p) -> t c p", p=128), lsb[i][:])

    cnt = dict(s=0, o=0, p=0, pt=0, ob=0, bq=0)
    for kind in range(2):
        base = 0 if kind == 0 else 1544
        for hp in range(4):
            u = kind * 4 + hp
            for (wt, off, nm) in ((wq, 0, "wq"), (wk, 512, "wk"), (wv, 1024, "wv")):
                c0 = base + off + hp * 128
                S.dma([], [nm], wt[:], w_in[:, c0:c0 + 128].rearrange("(kc p) n -> p kc n", p=128), q="pool")
            if kind == 1:
                S.dma([], ["dilB"], dilB[:], C.cd["dil_bias"][:, hp * 2:hp * 2 + 2, :, :], q="pool")
                if hp == 0:
                    for hh in range(2):
                        zr = slice(64, 128) if hh == 0 else slice(0, 64)
                        S.op("pool", [], [("KA", hh)], nc.gpsimd.memset, KA[hh][zr, :], 0.0)
                        S.op("pool", [], [("QA", hh)], nc.gpsimd.memset, QA[hh][zr, :], 0.0)
            else:
                for hh in range(2):
                    S.dma([("lrow", hp * 2 + hh)], [("KA", hh)], KA[hh][BR[hh], :], C.lrow_d[hp * 2 + hh, 0])
                    S.dma([("lrow", hp * 2 + hh)], [("QA", hh)], QA[hh][BR[hh], :], C.lrow_d[hp * 2 + hh, 1])
            for (wt, nm, dst, dn) in ((wq, "wq", QA, "QA"), (wk, "wk", KA, "KA")):
                for tb in range(8):
                    pi = cnt["p"] % 2
                    cnt["p"] += 1
                    for kc in range(KC):
                        S.op("pe", [nm], [("psp", pi)], nc.tensor.matmul, ps_p[pi][:],
                             wt[:, kc, :], C.xT[:, kc, tb * 512:(tb + 1) * 512],
                             start=(kc == 0), stop=(kc == KC - 1))
                    for hh in range(2):
                        if dn == "QA":
                            S.op("act", [("psp", pi)], [(dn, hh)], nc.scalar.mul, dst[hh][FR[hh], tb * 512:(tb + 1) * 512],
                                 ps_p[pi][FR[hh], :], 0.125)
                        else:
                            S.op("dve", [("psp", pi)], [(dn, hh)], nc.vector.tensor_copy, dst[hh][FR[hh], tb * 512:(tb + 1) * 512],
                                 ps_p[pi][FR[hh], :])
            for t4 in range(NT // 4):
                pi = cnt["p"] % 2
                cnt["p"] += 1
                for j in range(4):
                    t = t4 * 4 + j
                    for kc in range(KC):
                        S.op("pe", ["wv"], [("psp", pi)], nc.tensor.matmul, ps_p[pi][:, j * 128:(j + 1) * 128],
                             C.xT[:, kc, t * 128:(t + 1) * 128], wv[:, kc, :],
                             start=(kc == 0), stop=(kc == KC - 1))
                S.op("dve", [("psp", pi)], ["vsb"], nc.vector.tensor_copy, vsb[:, t4 * 4:(t4 + 1) * 4, :, 0:64],
                     ps_p[pi][:].rearrange("p (a b c) -> p a b c", a=4, b=2))
            if kind == 0:
                for G in range(NT // 4):
                    gi = cnt["ob"] % 2
                    cnt["ob"] += 1
                    for hh in range(2):
                        h = hp * 2 + hh
                        hb = hh * 64
                        nk = 4 * G + 4
                        for j in range(nk):
                            si = cnt["s"] % 2
                            cnt["s"] += 1
                            ps = ps_s[si]
                            i0 = max(0, j - 4 * G)
                            c0 = i0 * 128
                            S.op("pe", [("KA", hh), ("QA", hh)], [("pss", si)], nc.tensor.matmul, ps[:, c0:512],
                                 KA[hh][:, j * 128:(j + 1) * 128], QA[hh][:, G * 512 + c0:(G + 1) * 512],
                                 start=True, stop=(j < 4 * G))
                            if j >= 4 * G:
                                S.op("pe", ["maskb", "identb"], [("pss", si)], nc.tensor.matmul, ps[:, c0:c0 + 128],
                                     C.identb[:], maskb[:], start=False, stop=True)
                            S.op("act", [("pss", si)], [("pTall", j)], nc.scalar.activation, pTall[:, j, c0:512],
                                 ps[:, c0:512], AF.Exp)
                        po_i = cnt["o"] % 2
                        cnt["o"] += 1
                        po = ps_o[po_i]
                        for j in range(nk):
                            c0 = max(0, j - 4 * G) * 128
                            S.op("pe", [("pTall", j), "vsb"], [("pso", po_i)], nc.tensor.matmul, po[0:65, c0:512],
                                 vsb[:, j, hh, :], pTall[:, j, c0:512], start=(j == 0), stop=(j == nk - 1), skip_group_check=True)
                        finalize(po, po_i, u, hh, G)
                continue
            for G in range(NT // 4):
                gi = cnt["ob"] % 2
                cnt["ob"] += 1
                for hh in range(2):
                    hb = hh * 64
                    jlo = max(0, 4 * G - (NDEL - 1))
                    for j in range(jlo, 4 * G + 4):
                        si = cnt["s"] % 2
                        cnt["s"] += 1
                        ps = ps_s[si]
                        i0 = max(0, j - 4 * G)
                        i1 = min(3, j + (NDEL - 1) - 4 * G)
                        c0, c1 = i0 * 128, (i1 + 1) * 128
                        d0 = 4 * G + i0 - j
                        S.op("pe", [("KA", hh), ("QA", hh)], [("pss", si)], nc.tensor.matmul, ps[:, c0:c1],
                             KA[hh][:, j * 128:(j + 1) * 128], QA[hh][:, G * 512 + c0:G * 512 + c1],
                             start=True, stop=False)
                        S.op("pe", ["dilB", "identb"], [("pss", si)], nc.tensor.matmul, ps[:, c0:c1], C.identb[:],
                             dilB[:, hh, d0:d0 + (i1 - i0 + 1), :].rearrange("p a b -> p (a b)"), start=False, stop=True)
                        S.op("act", [("pss", si)], [("pTall", j - jlo)], nc.scalar.activation, pTall[:, j - jlo, c0:c1],
                             ps[:, c0:c1], AF.Exp)
                    po_i = cnt["o"] % 2
                    cnt["o"] += 1
                    po = ps_o[po_i]
                    js = list(range(jlo, 4 * G + 4))
                    for j in js:
                        i0 = max(0, j - 4 * G)
                        i1 = min(3, j + (NDEL - 1) - 4 * G)
                        c0, c1 = i0 * 128, (i1 + 1) * 128
                        S.op("pe", [("pTall", j - jlo), "vsb"], [("pso", po_i)], nc.tensor.matmul, po[0:65, c0:c1],
                             vsb[:, j, hh, :], pTall[:, j - jlo, c0:c1], start=(j == js[0]), stop=(j == js[-1]), skip_group_check=True)
                    finalize(po, po_i, u, hh, G)


def alloc_router(C):
    S, nc = C.S, C.nc
    C.rt_w = S.sbuf("rtw", [128, KC, NE], F32)
    S.dma([], ["rtw"], C.rt_w[:], C.ind["moe_router"].rearrange("(kc p) n -> p kc n", p=128))
    C.rt_xT = S.sbuf("rtxT", [128, KC, 128], F32)
    C.rt_ps = [S.psum("rtps", [128, 512]) for _ in range(2)]
    C.rt_pl = S.psum("rtpl", [128, 8])
    C.rt_sm = S.sbuf("rtsm", [128, 6, 8], F32)


def router_tile(C, t, zn, znkey):
    S, nc = C.S, C.nc
    for g in range(2):
        for j in range(4):
            kc = g * 4 + j
            S.op("pe", [znkey, "ident"], [("rtps", g)], nc.tensor.transpose,
                 C.rt_ps[g][:, j * 128:(j + 1) * 128], zn[:, kc * 128:(kc + 1) * 128], C.ident[:])
        S.op("act", [("rtps", g)], ["rtxT"], nc.scalar.copy, C.rt_xT[:, g * 4:(g + 1) * 4, :],
             C.rt_ps[g][:].rearrange("p (j c) -> p j c", j=4))
    for kc in range(KC):
        S.op("pe", ["rtxT", "rtw"], ["rtpl"], nc.tensor.matmul, C.rt_pl[:], C.rt_xT[:, kc, :], C.rt_w[:, kc, :],
             start=(kc == 0), stop=(kc == KC - 1))
    sm = C.rt_sm
    lg, srt, msk, ex, nv, den = (sm[:, i, :] for i in range(6))
    S.op("dve", ["rtpl"], ["rtsm"], nc.vector.tensor_copy, lg, C.rt_pl[:])
    S.op("dve", ["rtsm"], ["rtsm"], nc.vector.max, srt, lg)
    S.op("dve", ["rtsm"], ["rtsm"], nc.vector.tensor_scalar, msk, lg, sm[:, 1, 1:2], None, ALU.is_ge)
    S.op("dve", ["rtsm"], ["rtsm"], nc.vector.tensor_scalar, nv[:, 0:1], sm[:, 1, 0:1], -1.0, None, ALU.mult)
    S.op("act", ["rtsm"], ["rtsm"], nc.scalar.activation, ex, lg, AF.Exp, bias=sm[:, 4, 0:1], scale=1.0)
    S.op("dve", ["rtsm"], ["rtsm"], nc.vector.tensor_tensor, ex, ex, msk, ALU.mult)
    S.op("dve", ["rtsm"], ["rtsm"], nc.vector.reduce_sum, den[:, 0:1], ex, mybir.AxisListType.X)
    S.op("dve", ["rtsm"], ["rtsm"], nc.vector.reciprocal, den[:, 1:2], den[:, 0:1])
    S.op("dve", ["rtsm"], ["gate"], nc.vector.tensor_scalar, C.gate[:, t, :], ex, sm[:, 5, 1:2], None, ALU.mult)


def stage_gdn(C):
    import os
    S, nc, ind = C.S, C.nc, C.ind
    w_in = ind["gdn_w_in"]
    X = mybir.AxisListType.X
    sb = lambda n, shp, dt=F32: S.sbuf(n, shp, dt)
    onecol = sb("onecol", [128, 1]); triu = sb("triu", [128, 128]); ones = sb("ones", [128, 128])
    nm1 = sb("nm1", [128, 128]); nm2 = sb("nm2", [128, 128])
    S.dma([], ["triu"], triu[:], C.cd["tri_u"]); S.dma([], ["ones"], ones[:], C.cd["ones"])
    S.dma([], ["nm1"], nm1[:], C.cd["gdn_m1"]); S.dma([], ["nm2"], nm2[:], C.cd["gdn_m2"])
    S.op("pool", [], ["onecol"], nc.gpsimd.memset, onecol[:], 1.0)
    dtbB = load_bcast(C, "dtbB", ind["gdn_dt_bias"], 8)
    alogB = load_bcast(C, "alogB", ind["gdn_a_log"], 8)
    ngB = load_bcast(C, "ngB", ind["gdn_norm_g"], 128)
    wba = sb("wba", [128, KC, 16], BF16)
    S.dma([], ["wba"], wba[:], w_in[:, 3072:3088].rearrange("(kc p) n -> p kc n", p=128), q="pool")
    cwr = sb("cwr", [96, 128]); cw = sb("cw", [128, 96])
    S.dma([], ["cwr"], cwr[:], ind["gdn_conv_w"].rearrange("j (c p) -> (j c) p", p=128))
    P = [S.psum("gp", [128, 512]) for _ in range(7)]
    TBM = S.psum("gtb", [128, 512], BF16)
    TBs = [TBM, TBM]
    pk = lambda i: ("gp", i)
    S.op("pe", ["cwr", "ident"], [pk(0)], nc.tensor.transpose, P[0][:, 0:96], cwr[:], C.ident[0:96, 0:96])
    S.op("dve", [pk(0)], ["cw"], nc.vector.tensor_copy, cw[:], P[0][:, 0:96])
    names = ["braw", "araw", "beta", "gneg", "Gc", "Gl", "egam", "egl", "gtot"]
    sc = {n: sb(n, [128, NT, 8]) for n in names}
    fl = lambda n: sc[n][:].rearrange("p a b -> p (a b)")
    S.op("act", ["alogB"], ["alogB"], nc.scalar.activation, alogB[:], alogB[:], AF.Exp)
    for t in range(NT):
        pi = t % 2
        for kc in range(KC):
            S.op("pe", ["wba"], [pk(pi)], nc.tensor.matmul, P[pi][:, 0:16], C.xT[:, kc, t * 128:(t + 1) * 128],
                 wba[:, kc, :], start=(kc == 0), stop=(kc == KC - 1))
        S.op("dve", [pk(pi)], ["braw"], nc.vector.tensor_copy, sc["braw"][:, t, :], P[pi][:, 0:8])
        S.op("dve", [pk(pi), "dtbB"], ["araw"], nc.vector.tensor_tensor, sc["araw"][:, t, :], P[pi][:, 8:16], dtbB[:], ALU.add)
        S.op("dve", ["araw"], ["araw"], nc.vector.tensor_copy, sc["araw"][:, t, :], sc["araw"][:, t, :]) if False else None
    S.op("act", ["braw"], ["braw"], nc.scalar.activation, fl("braw"), fl("braw"), AF.Exp, scale=-1.0)
    S.op("dve", ["braw"], ["braw"], nc.vector.tensor_scalar, fl("braw"), fl("braw"), 1.0, None, ALU.add)
    S.op("dve", ["braw"], ["beta"], nc.vector.reciprocal, fl("beta"), fl("braw"))
    S.op("act", ["araw"], ["araw"], nc.scalar.activation, fl("araw"), fl("araw"), AF.Exp)
    S.op("act", ["araw", "onecol"], ["araw"], nc.scalar.activation, fl("araw"), fl("araw"), AF.Ln, bias=onecol[:, 0:1], scale=1.0)
    for t in range(NT):
        S.op("dve", ["araw", "alogB"], ["gneg"], nc.vector.tensor_tensor, sc["gneg"][:, t, :], sc["araw"][:, t, :], alogB[:], ALU.mult)
    S.op("pe", ["gneg", "triu"], [pk(0)], nc.tensor.matmul, P[0][:, 0:256], triu[:], fl("gneg"), start=True, stop=True)
    S.op("pe", ["gneg", "ones"], [pk(1)], nc.tensor.matmul, P[1][:, 0:256], ones[:], fl("gneg"), start=True, stop=True)
    S.op("dve", [pk(0)], ["Gc"], nc.vector.tensor_copy, fl("Gc"), P[0][:, 0:256])
    S.op("dve", [pk(1)], ["Gl"], nc.vector.tensor_copy, fl("Gl"), P[1][:, 0:256])
    S.op("act", ["Gc"], ["egam"], nc.scalar.activation, fl("egam"), fl("Gc"), AF.Exp, scale=-1.0)
    S.op("act", ["Gl"], ["gtot"], nc.scalar.activation, fl("gtot"), fl("Gl"), AF.Exp, scale=-1.0)
    S.op("dve", ["Gc", "Gl"], ["egl"], nc.vector.tensor_tensor, fl("egl"), fl("Gc"), fl("Gl"), ALU.subtract)
    S.op("act", ["egl"], ["egl"], nc.scalar.activation, fl("egl"), fl("egl"), AF.Exp)

    wqkv = [sb("wqkv", [128, KC, 128], BF16) for _ in range(3)]
    wgt = sb("wgt", [128, KC, 128], BF16)
    hpre = sb("hpre", [128, SEQ + 4], BF16)
    fT = [sb("fT", [128, SEQ], BF16) for _ in range(3)]
    dg = sb("dg", [128, 12, 128], BF16)
    ktm = sb("ktm", [128, NT, 128], BF16); vb = sb("vb", [128, NT, 128], BF16)
    kbg = sb("kbg", [128, NT, 128], BF16); kdec = ktm
    qkT = sb("qkT", [128, NT, 128], BF16)
    usb = sb("usb", [128, NT, 128], BF16)
    hs = {n: sb("hs_" + n, [128, NT]) for n in ["ssqk", "ssqq", "lnk", "lnq", "rk", "rq", "r1", "r2", "nGc",
                                                 "skbg", "skdec", "sqg", "nbrk"]}
    junk = sb("junk", [128, 128])
    junk2 = sb("junk2", [128, 128])
    qtm = [sb("qtm", [128, 128], BF16) for _ in range(2)]
    d12 = [sb("d12", [128, 2, 128]) for _ in range(2)]
    E12 = [sb("E12", [128, 2, 128]) for _ in range(2)]
    Mb = [[[sb("Mb", [128, 128], BF16) for _ in range(2)] for _ in range(4)] for _ in range(2)]
    Mtb = [[[sb("Mtb", [128, 128], BF16) for _ in range(2)] for _ in range(4)] for _ in range(2)]
    Yb = [[[sb("Yb", [128, 128], BF16) for _ in range(2)] for _ in range(4)] for _ in range(2)]
    Sf = sb("Sf", [128, 128]); Sb = sb("Sb", [128, 128], BF16)
    vnew = [sb("vnew", [128, 128], BF16) for _ in range(2)]
    o1 = [sb("o1", [128, 128]) for _ in range(2)]
    sgall = sb("sgall", [128, NT, 128], BF16)
    og = [sb("og", [128, 128], BF16) for _ in range(2)]
    osm = [sb("osm", [128, 4]) for _ in range(2)]
    S.op("pool", [], ["hpre"], nc.gpsimd.memset, hpre[:, 0:4], 0.0)
    wT = fT[2][:].rearrange("p (a b) -> p a b", a=NT)

    import os
    STOP = os.environ.get("GDN_STOP", "")
    if STOP == "A":
        return
    for h in range(8 if not STOP else 1):
        Gc_h, beta_h = sc["Gc"][:, :, h], sc["beta"][:, :, h]
        egam_h, egl_h = sc["egam"][:, :, h], sc["egl"][:, :, h]
        for ci in range(3):
            col = ci * 1024 + h * 128
            S.dma([], [("wqkv", ci)], wqkv[ci][:], w_in[:, col:col + 128].rearrange("(kc p) n -> p kc n", p=128), q="pool")
        S.dma([], ["wgt"], wgt[:], w_in[:, 3088 + h * 128:3088 + (h + 1) * 128].rearrange("(kc p) n -> p kc n", p=128), q="pool")
        for ci in range(3):
            for j in range(4):
                idx = j * 24 + ci * 8 + h
                S.op("dve", ["cw", "ident"], ["dg"], nc.vector.tensor_scalar, dg[:, ci * 4 + j, :], C.ident[:],
                     cw[:, idx:idx + 1], None, ALU.mult)
        for ci in range(3):
            for tb in range(8):
                pi = tb % 2
                for kc in range(KC):
                    S.op("pe", [("wqkv", ci)], [pk(pi)], nc.tensor.matmul, P[pi][:], wqkv[ci][:, kc, :],
                         C.xT[:, kc, tb * 512:(tb + 1) * 512], start=(kc == 0), stop=(kc == KC - 1))
                S.op("dve", [pk(pi)], ["hpre"], nc.vector.tensor_copy, hpre[:, 4 + tb * 512:4 + (tb + 1) * 512], P[pi][:])
            for tb in range(8):
                pi = 2 + tb % 2
                for j in range(4):
                    S.op("pe", ["hpre", "dg"], [pk(pi)], nc.tensor.matmul, P[pi][:], dg[:, ci * 4 + j, :],
                         hpre[:, 1 + tb * 512 + j:1 + tb * 512 + j + 512], start=(j == 0), stop=(j == 3))
                S.op("act", [pk(pi)], [("fT", ci)], nc.scalar.activation, fT[ci][:, tb * 512:(tb + 1) * 512], P[pi][:], AF.Silu)
        if STOP == "B1":
            return
        for t in range(NT):
            sl = slice(t * 128, (t + 1) * 128)
            pi = ("tbm",)
            o0 = 0
            TB = TBs[t % 2]
            S.op("pe", [("fT", 0), "identb"], [pi], nc.tensor.transpose, TB[:, o0:o0 + 128], fT[0][:, sl], C.identb[:])
            S.op("pe", [("fT", 1), "identb"], [pi], nc.tensor.transpose, TB[:, o0 + 128:o0 + 256], fT[1][:, sl], C.identb[:])
            S.op("pe", [("fT", 2), "identb"], [pi], nc.tensor.transpose, TB[:, o0 + 256:o0 + 384], fT[2][:, sl], C.identb[:])
            LV = int(os.environ.get("B2LV", "9"))
            if LV >= 1:
                S.op("dve", [pi], [("qtm", t % 2)], nc.vector.tensor_copy, qtm[t % 2][:], TB[:, o0:o0 + 128])
                S.op("dve", [pi], ["ktm"], nc.vector.tensor_copy, ktm[:, t, :], TB[:, o0 + 128:o0 + 256])
            if LV >= 2:
                S.op("dve", [pi, "beta"], ["vb"], nc.vector.tensor_scalar, vb[:, t, :], TB[:, o0 + 256:o0 + 384],
                     sc["beta"][:, t, h:h + 1], None, ALU.mult)
            if LV >= 3:
                S.op("act", [("qtm", t % 2)], ["junk"], nc.scalar.activation, junk[:], qtm[t % 2][:], AF.Square)
                S.op("act", ["ktm"], ["junk2"], nc.scalar.activation, junk2[:], ktm[:, t, :], AF.Square)
            if LV >= 4:
                S.op("dve", ["junk"], ["ssqq"], nc.vector.reduce_sum, hs["ssqq"][:, t:t + 1], junk[:], X)
                S.op("dve", ["junk2"], ["ssqk"], nc.vector.reduce_sum, hs["ssqk"][:, t:t + 1], junk2[:], X)
        if STOP == "B2":
            return
        H = lambda n: hs[n][:]
        S.op("act", ["ssqk", "epsln"], ["lnk"], nc.scalar.activation, H("lnk"), H("ssqk"), AF.Ln, bias=C.eps_ln[:, 1:2], scale=1.0)
        S.op("act", ["ssqq", "epsln"], ["lnq"], nc.scalar.activation, H("lnq"), H("ssqq"), AF.Ln, bias=C.eps_ln[:, 1:2], scale=1.0)
        S.op("act", ["lnk"], ["rk"], nc.scalar.activation, H("rk"), H("lnk"), AF.Exp, scale=-0.5)
        S.op("act", ["lnq"], ["rq"], nc.scalar.activation, H("rq"), H("lnq"), AF.Exp, scale=-0.5)
        S.op("dve", ["lnk", "Gc"], ["r1"], nc.vector.scalar_tensor_tensor, H("r1"), H("lnk"), -0.5, Gc_h, ALU.mult, ALU.add)
        S.op("dve", ["lnq", "Gc"], ["r2"], nc.vector.scalar_tensor_tensor, H("r2"), H("lnq"), -0.5, Gc_h, ALU.mult, ALU.subtract)
        S.op("dve", ["r2"], ["r2"], nc.vector.tensor_scalar, H("r2"), H("r2"), -0.5 * math.log(128.0), None, ALU.add)
        S.op("dve", ["Gc"], ["nGc"], nc.vector.tensor_scalar, H("nGc"), Gc_h, -1.0, None, ALU.mult)
        S.op("dve", ["rk", "beta"], ["skbg"], nc.vector.tensor_tensor, H("skbg"), H("rk"), beta_h, ALU.mult)
        S.op("dve", ["skbg", "egam"], ["skbg"], nc.vector.tensor_tensor, H("skbg"), H("skbg"), egam_h, ALU.mult)
        S.op("dve", ["rk", "egl"], ["skdec"], nc.vector.tensor_tensor, H("skdec"), H("rk"), egl_h, ALU.mult)
        S.op("dve", ["rq", "egam"], ["sqg"], nc.vector.scalar_tensor_tensor, H("sqg"), H("rq"), 128.0 ** -0.5, egam_h, ALU.mult, ALU.mult)
        S.op("dve", ["rk", "beta"], ["nbrk"], nc.vector.scalar_tensor_tensor, H("nbrk"), beta_h, -1.0, H("rk"), ALU.mult, ALU.mult)
        for t in range(NT):
            S.op("dve", ["ktm", "skbg"], ["kbg"], nc.vector.tensor_scalar, kbg[:, t, :], ktm[:, t, :], hs["skbg"][:, t:t + 1], None, ALU.mult)
            S.op("dve", ["ktm", "skdec"], ["ktm"], nc.vector.tensor_scalar, kdec[:, t, :], ktm[:, t, :], hs["skdec"][:, t:t + 1], None, ALU.mult)
        if STOP == "B3":
            return
        def b4_setup_tile(t0, par, b4):
            t = t0 + b4
            sl = slice(t * 128, (t + 1) * 128)
            cs = slice(b4 * 128, (b4 + 1) * 128)
            di = t % 2
            S.op("dve", ["r1", "nm1"], [("d12", di)], nc.vector.tensor_scalar, d12[di][:, 0, :], nm1[:], hs["r1"][:, t:t + 1], None, ALU.add)
            S.op("dve", ["r2", "nm2"], [("d12", di)], nc.vector.tensor_scalar, d12[di][:, 1, :], nm2[:], hs["r2"][:, t:t + 1], None, ALU.add)
            S.op("pe", [("fT", 1)], [pk(0)], nc.tensor.matmul, P[0][:, cs], fT[1][:, sl], fT[1][:, sl], start=True, stop=True)
            S.op("pe", [("fT", 1), ("fT", 0)], [pk(1)], nc.tensor.matmul, P[1][:, cs], fT[1][:, sl], fT[0][:, sl], start=True, stop=True)
            S.op("pe", [("d12", di), "ident"], [pk(2)], nc.tensor.transpose, P[2][:, cs], d12[di][:, 0, :], C.ident[:])
            S.op("pe", [("d12", di), "ident"], [pk(3)], nc.tensor.transpose, P[3][:, cs], d12[di][:, 1, :], C.ident[:])
            S.op("act", [pk(2), "nGc"], [("E12", di)], nc.scalar.activation, E12[di][:, 0, :], P[2][:, cs], AF.Exp,
                 bias=hs["nGc"][:, t:t + 1], scale=1.0)
            S.op("act", [pk(3), "Gc"], [("E12", di)], nc.scalar.activation, E12[di][:, 1, :], P[3][:, cs], AF.Exp,
                 bias=sc["Gc"][:, t, h:h + 1], scale=1.0)
            S.op("dve", [pk(0), ("E12", di), "nbrk"], [("Mb", par, b4, 0)], nc.vector.scalar_tensor_tensor, Mb[par][b4][0][:],
                 P[0][:, cs], hs["nbrk"][:, t:t + 1], E12[di][:, 0, :], ALU.mult, ALU.mult)
            S.op("dve", [pk(1), ("E12", di), "rk"], ["qkT"], nc.vector.scalar_tensor_tensor, qkT[:, t, :],
                 P[1][:, cs], hs["rk"][:, t:t + 1], E12[di][:, 1, :], ALU.mult, ALU.mult)
            S.op("pe", [("Mb", par, b4, 0), "identb"], [("tbm",)], nc.tensor.transpose, TBM[:, cs], Mb[par][b4][0][:], C.identb[:])
            S.op("dve", [("tbm",)], [("Mtb", par, b4, 0)], nc.vector.tensor_copy, Mtb[par][b4][0][:], TBM[:, cs])
            S.op("dve", [("Mtb", par, b4, 0), "identb"], [("Yb", par, b4, 0)], nc.vector.tensor_tensor, Yb[par][b4][0][:],
                 Mtb[par][b4][0][:], C.identb[:], ALU.add)

        def b4_level(par, lvl, cur):
            nxt = 1 - cur
            for b4 in range(4):
                cs = slice(b4 * 128, (b4 + 1) * 128)
                S.op("pe", [("Mtb", par, b4, cur), ("Mb", par, b4, cur)], [pk(4)], nc.tensor.matmul, P[4][:, cs],
                     Mtb[par][b4][cur][:], Mb[par][b4][cur][:], start=True, stop=True)
                if lvl < 6:
                    S.op("pe", [("Mtb", par, b4, cur), ("Mb", par, b4, cur)], [pk(5)], nc.tensor.matmul, P[5][:, cs],
                         Mb[par][b4][cur][:], Mtb[par][b4][cur][:], start=True, stop=True)
            for b4 in range(4):
                cs = slice(b4 * 128, (b4 + 1) * 128)
                S.op("act", [pk(4)], [("Mb", par, b4, nxt)], nc.scalar.copy, Mb[par][b4][nxt][:], P[4][:, cs])
                if lvl < 6:
                    S.op("dve", [pk(5)], [("Mtb", par, b4, nxt)], nc.vector.tensor_copy, Mtb[par][b4][nxt][:], P[5][:, cs])
            for b4 in range(4):
                cs = slice(b4 * 128, (b4 + 1) * 128)
                S.op("pe", [("Mb", par, b4, nxt), ("Yb", par, b4, cur)], [pk(6)], nc.tensor.matmul, P[6][:, cs],
                     Mb[par][b4][nxt][:], Yb[par][b4][cur][:], start=True, stop=True)
            for b4 in range(4):
                cs = slice(b4 * 128, (b4 + 1) * 128)
                S.op("dve", [pk(6), ("Yb", par, b4, cur)], [("Yb", par, b4, nxt)], nc.vector.tensor_tensor, Yb[par][b4][nxt][:],
                     Yb[par][b4][cur][:], P[6][:, cs], ALU.add)
            return nxt

        def b4_tail(t0, par, cur):
            for b4 in range(4):
                t = t0 + b4
                cs = slice(b4 * 128, (b4 + 1) * 128)
                S.op("pe", [("Yb", par, b4, cur), "vb"], [pk(4)], nc.tensor.matmul, P[4][:, cs], Yb[par][b4][cur][:], vb[:, t, :], start=True, stop=True)
                S.op("pe", [("Yb", par, b4, cur), "kbg"], [pk(5)], nc.tensor.matmul, P[5][:, cs], kbg[:, t, :], Yb[par][b4][cur][:], start=True, stop=True)
            S.op("act", [pk(4)], ["usb"], nc.scalar.copy, usb[:, t0:t0 + 4, :], P[4][:].rearrange("p (a b) -> p a b", a=4))
            S.op("dve", [pk(5)], [("fT", 2)], nc.vector.tensor_copy, wT[:, t0:t0 + 4, :], P[5][:].rearrange("p (a b) -> p a b", a=4))

        for b4 in range(4):
            b4_setup_tile(0, 0, b4)
        for n in range(NT // 4):
            par = n % 2
            cur = 0
            for lvl in range(1, 7):
                cur = b4_level(par, lvl, cur)
                if n + 1 < NT // 4 and lvl <= 4:
                    b4_setup_tile((n + 1) * 4, 1 - par, lvl - 1)
            b4_tail(n * 4, par, cur)
        if STOP == "B4":
            return
        for t4 in range(NT // 4):
            gp = 4 + t4 % 2
            for j in range(4):
                t = t4 * 4 + j
                for kc in range(KC):
                    S.op("pe", ["wgt"], [pk(gp)], nc.tensor.matmul, P[gp][:, j * 128:(j + 1) * 128],
                         C.xT[:, kc, t * 128:(t + 1) * 128], wgt[:, kc, :], start=(kc == 0), stop=(kc == KC - 1))
            S.op("act", [pk(gp)], ["sgall"], nc.scalar.activation, sgall[:, t4 * 4:(t4 + 1) * 4, :],
                 P[gp][:].rearrange("p (a b) -> p a b", a=4), AF.Silu)
        S.op("pool", [], ["Sf"], nc.gpsimd.memset, Sf[:], 0.0)
        S.op("pool", [], ["Sb"], nc.gpsimd.memset, Sb[:], 0.0)
        for t in range(NT):
            sl = slice(t * 128, (t + 1) * 128)
            i2 = t % 2
            S.op("pe", [("fT", 2), "Sb"], [pk(2)], nc.tensor.matmul, P[2][:, 0:128], wT[:, t, :], Sb[:], start=True, stop=True)
            S.op("pe", [("fT", 0), "Sb"], [pk(2)], nc.tensor.matmul, P[2][:, 128:256], fT[0][:, sl], Sb[:], start=True, stop=True)
            B5S = int(os.environ.get("B5S", "9"))
            if B5S >= 1:
                S.op("dve", [pk(2), "usb"], [("vnew", i2)], nc.vector.tensor_tensor, vnew[i2][:], usb[:, t, :], P[2][:, 0:128], ALU.subtract)
            if B5S >= 2:
                S.op("dve", [pk(2), "sqg"], [("o1", i2)], nc.vector.tensor_scalar, o1[i2][:], P[2][:, 128:256], hs["sqg"][:, t:t + 1], None, ALU.mult)
            B5 = int(os.environ.get("B5LV", "9"))
            if B5 < 2:
                continue
            S.op("pe", ["qkT", ("vnew", i2)], [pk(3)], nc.tensor.matmul, P[3][:, 0:128], qkT[:, t, :], vnew[i2][:], start=True, stop=True)
            S.op("pe", ["ktm", ("vnew", i2)], [pk(3)], nc.tensor.matmul, P[3][:, 128:256], kdec[:, t, :], vnew[i2][:], start=True, stop=True)
            S.op("dve", [pk(3), ("o1", i2)], [("o1", i2)], nc.vector.tensor_tensor, o1[i2][:], o1[i2][:], P[3][:, 0:128], ALU.add)
            S.op("dve", [pk(3), "Sf", "gtot"], ["Sb"], nc.vector.scalar_tensor_tensor, Sb[:], Sf[:], sc["gtot"][:, t, h:h + 1],
                 P[3][:, 128:256], ALU.mult, ALU.add)
            S.op("dve", [pk(3), "Sf", "gtot"], ["Sf"], nc.vector.scalar_tensor_tensor, Sf[:], Sf[:], sc["gtot"][:, t, h:h + 1],
                 P[3][:, 128:256], ALU.mult, ALU.add)
            if B5 < 3:
                continue
            if B5 < 4:
                continue
            S.op("act", [("o1", i2)], ["junk"], nc.scalar.activation, junk[:], o1[i2][:], AF.Square)
            S.op("dve", ["junk"], [("osm", i2)], nc.vector.reduce_sum, osm[i2][:, 0:1], junk[:], X)
            S.op("act", [("osm", i2), "epsln"], [("osm", i2)], nc.scalar.activation, osm[i2][:, 1:2], osm[i2][:, 0:1], AF.Sqrt,
                 bias=C.eps_ln[:, 1:2], scale=1.0 / 128.0)
            S.op("dve", [("osm", i2)], [("osm", i2)], nc.vector.reciprocal, osm[i2][:, 2:3], osm[i2][:, 1:2])
            S.op("dve", [("o1", i2), ("osm", i2), "ngB"], [("o1", i2)], nc.vector.scalar_tensor_tensor, o1[i2][:], o1[i2][:],
                 osm[i2][:, 2:3], ngB[:], ALU.mult, ALU.mult)
            if B5 < 5:
                continue
            S.op("pool", [("o1", i2), "sgall"], [("og", i2)], nc.gpsimd.tensor_tensor, og[i2][:], o1[i2][:], sgall[:, t, :], ALU.mult)
            S.dma([("og", i2)], [("o_d", t, h)], C.o_d[t * 128:(t + 1) * 128, h * 128:(h + 1) * 128], og[i2][:])


def stage_load_oT(C):
    S, nc = C.S, C.nc
    with S.scope():
        ob = [S.sbuf("ob", [128, D], BF16) for _ in range(2)]
        pst = [S.psum("pstro", [128, 512], BF16) for _ in range(2)]
        for t in range(NT):
            i = t % 2
            S.dma([("o_d", t, h) for h in range(8)] + [("o_d", t)], [("ob", i)], ob[i][:], C.o_d[t * 128:(t + 1) * 128, :])
            for g in range(2):
                for j in range(4):
                    kc = g * 4 + j
                    S.op("pe", [("ob", i), "identb"], [("pstro", g)], nc.tensor.transpose,
                         pst[g][:, j * 128:(j + 1) * 128], ob[i][:, kc * 128:(kc + 1) * 128], C.identb[:])
                S.op("dve", [("pstro", g)], [("oTall",)], nc.vector.tensor_copy,
                     C.oT[:, g * 4:(g + 1) * 4, t * 128:(t + 1) * 128], pst[g][:].rearrange("p (j c) -> p j c", j=4))


def build(stages, dbg_in=None, dbg_out=None):
    nc = bass.Bass("TRN2", target_bir_lowering=False)
    C = Ctx()
    C.nc = nc
    C.S = S = Sched(nc)
    ind = {k: nc.dram_tensor(k, v, F32, kind="ExternalInput").ap() for k, v in IN_SHAPES.items()}
    C.cd = {k: nc.dram_tensor("c_" + k, v, F32, kind="ExternalInput").ap() for k, v in CONST_SHAPES.items()}
    out_d = nc.dram_tensor("out", [SEQ, D], F32, kind="ExternalOutput").ap()
    C.ind = ind

    def scratch(name, shape, dt=F32):
        if dbg_in == name:
            return nc.dram_tensor(name, shape, dt, kind="ExternalInput").ap()
        if dbg_out == name:
            return nc.dram_tensor(name, shape, dt, kind="ExternalOutput").ap()
        return nc.dram_tensor(name, shape, dt).ap()

    C.x1_d = scratch("x1", [SEQ, D])
    C.x2_d = scratch("x2", [SEQ, D])
    C.x3_d = scratch("x3", [SEQ, D])
    C.yacc_d = scratch("yacc", [SEQ, D])
    C.hT_d = [nc.dram_tensor("hT%d" % i, [16, 128, NFC, 256], BF16).ap() for i in range(2)]
    C.o_d = nc.dram_tensor("o_d", [SEQ, D], BF16).ap()
    C.oT_d = nc.dram_tensor("oT_d", [D, SEQ], BF16).ap()
    C.lrow_d = nc.dram_tensor("lrow_d", [8, 2, 6, SEQ], BF16).ap()
    alloc_common(C)

    if "attn" in stages:
        stage_load_xT(C, ind["x"], "x")
        with S.scope():
            stage_attn(C)
        with S.scope():
            C.oT = S.sbuf("oT", [128, KC, SEQ], BF16)
            for kc in range(KC):
                S.dma([("oT_d", kc)], [("oTall",)], C.oT[:, kc, :], C.oT_d[kc * 128:(kc + 1) * 128, :])
            stage_outproj_ln(C, ind["attn_w_out"], ind["x"], "x", ind["ln_attn_g"], ind["ln_attn_b"], C.x1_d, "x1")
    if "ffn" in stages:
        if "attn" not in stages:
            stage_load_xT(C, C.x1_d, "x1")
        stage_ffn(C, [(ind["ffn_w_gate"], ind["ffn_w_up"], ind["ffn_w_down"])], C.x1_d, "x1",
                  ind["ln_ffn_g"], ind["ln_ffn_b"], C.x2_d, "x2")
    if "gdn" in stages:
        if "ffn" not in stages:
            stage_load_xT(C, C.x2_d, "x2")
        with S.scope():
            stage_gdn(C)
        with S.scope():
            C.oT = S.sbuf("oT", [128, KC, SEQ], BF16)
            stage_load_oT(C)
            stage_outproj_ln(C, ind["gdn_w_out"], C.x2_d, "x2", ind["ln_gdn_g"], ind["ln_gdn_b"], C.x3_d, "x3",
                             router=router_tile)
    if "moe" in stages:
        if "gdn" not in stages:
            with S.scope():
                alloc_ln(C)
                alloc_router(C)
                for t in range(NT):
                    i = t % 2
                    S.dma([("x3", t)], [("lnx", i)], C.lnx[i][:], C.x3_d[t * 128:(t + 1) * 128, :])
                    emit_xT(C, t, (C.lnx[i][:], ("lnx", i)))
                    router_tile(C, t, C.lnx[i], ("lnx", i))
        mg, mu, md = ind["moe_w_gate"], ind["moe_w_up"], ind["moe_w_down"]
        stage_ffn(C, [(mg[e], mu[e], md[e]) for e in range(NE)], C.x3_d, "x3",
                  ind["ln_moe_g"], ind["ln_moe_b"], out_d, "out", gate_sb=C.gate, make_xT=False)
    S.barrier()
    fin = {"ffn": C.x2_d, "attn": C.x1_d, "gdn": C.x3_d, "moe": None}[stages[-1]] if dbg_out is None else None
    if fin is not None:
        with S.scope():
            buf = [S.sbuf("fin", [128, D], F32) for _ in range(2)]
            for t in range(NT):
                i = t % 2
                S.dma([], [("fin", i)], buf[i][:], fin[t * 128:(t + 1) * 128, :])
                S.dma([("fin", i)], [("out", t)], out_d[t * 128:(t + 1) * 128, :], buf[i][:])
    S.barrier()
    print("n_inst", S.n_inst, S.cnt)
    S.close()
    return nc


def kernel(**inputs):
    nc = build(["attn", "ffn", "gdn", "moe"])
    consts = make_consts()
    shared = {}
    for k, shp in IN_SHAPES.items():
        if k == "x":
            continue
        shared[k] = np.ascontiguousarray(np.asarray(inputs[k], dtype=np.float32).reshape(shp))
    for k, v in consts.items():
        shared["c_" + k] = v
    x = np.asarray(inputs["x"], dtype=np.float32)
    in_maps = []
    for b in range(8):
        m = dict(shared)
        m["x"] = np.ascontiguousarray(x[b])
        in_maps.append(m)
    res = run_bass_kernel_spmd(nc, in_maps, core_ids=list(range(8)))
    return np.stack([np.asarray(r["out"], dtype=np.float32) for r in res.results], axis=0)
```

```python
import contextlib
import math
import numpy as np
import concourse.bass as bass
import concourse.mybir as mybir
from concourse.bass_utils import run_bass_kernel_spmd

F32 = mybir.dt.float32
BF16 = mybir.dt.bfloat16
AF = mybir.ActivationFunctionType
ALU = mybir.AluOpType

SEQ = 4096
D = 1024
NT = SEQ // 128
KC = D // 128
DFF = 3584
NFC = DFF // 128
NE = 8
ALPHA = 4.0 ** 0.25
LN_EPS = 1e-5
RMS_EPS = 1e-6
NEG = -30000.0
NDEL = 17


class Sched:
    def __init__(self, nc, n_dma_sems=24):
        self.nc = nc
        self.base = contextlib.ExitStack()
        self.stacks = [self.base]
        self.eng = {"pe": nc.tensor, "dve": nc.vector, "act": nc.scalar,
                    "pool": nc.gpsimd, "sp": nc.sync}
        self.sem, self.cnt = {}, {}
        for e in self.eng:
            self.sem[e] = self.base.enter_context(nc.semaphore("s_" + e))
            self.cnt[e] = 0
        self.dsem = [self.base.enter_context(nc.semaphore(f"d{i}")) for i in range(n_dma_sems)]
        self.dcnt = [0] * n_dma_sems
        self.dnext = 0
        self.dnext_sw = 0
        self.n_hw = n_dma_sems - 8
        self.seen = {e: {} for e in self.eng}
        self.lastw, self.reads = {}, {}
        self.n_inst = 0
        self._uid = 0

    def sbuf(self, name, shape, dt):
        self._uid += 1
        return self.stacks[-1].enter_context(self.nc.sbuf_tensor(f"{name}_{self._uid}", list(shape), dt))

    def psum(self, name, shape, dt=F32):
        self._uid += 1
        return self.stacks[-1].enter_context(self.nc.psum_tensor(f"{name}_{self._uid}", list(shape), dt))

    @contextlib.contextmanager
    def scope(self):
        st = contextlib.ExitStack()
        self.stacks.append(st)
        try:
            yield
        finally:
            self.barrier()
            self.stacks.pop()
            st.close()

    def _wait(self, e, ev):
        key, val, src = ev
        if src == e and e == "pe":
            return
        if self.seen[e].get(key, 0) >= val:
            return
        semh = self.sem[key] if isinstance(key, str) else self.dsem[key]
        self.eng[e].wait_ge(semh, val)
        self.seen[e][key] = val

    def _deps(self, e, reads, writes):
        for r in reads:
            ev = self.lastw.get(r)
            if ev is not None:
                self._wait(e, ev)
        for w in writes:
            ev = self.lastw.get(w)
            if ev is not None:
                self._wait(e, ev)
            for ev in self.reads.get(w, ()):
                self._wait(e, ev)

    def _commit(self, ev, reads, writes):
        for r in reads:
            self.reads.setdefault(r, []).append(ev)
        for w in writes:
            self.lastw[w] = ev
            self.reads[w] = []

    def op(self, e, reads, writes, fn, *a, **k):
        self._deps(e, reads, writes)
        inst = fn(*a, **k)
        self.cnt[e] += 1
        inst.then_inc(self.sem[e], 1)
        ev = (e, self.cnt[e], e)
        self._commit(ev, reads, writes)
        self.n_inst += 1
        return ev

    def dma(self, reads, writes, out, in_, q="sp", **k):
        if q == "pool":
            i = self.n_hw + self.dnext_sw
            self.dnext_sw = (self.dnext_sw + 1) % (len(self.dsem) - self.n_hw)
        else:
            i = self.dnext
            self.dnext = (self.dnext + 1) % self.n_hw
        if self.dcnt[i] > 0:
            self._wait(q, (i, self.dcnt[i], "dma"))
        self._deps(q, reads, writes)
        inst = self.eng[q].dma_start(out=out, in_=in_, **k)
        self.dcnt[i] += 16
        inst.then_inc(self.dsem[i], 16)
        ev = (i, self.dcnt[i], "dma")
        self._commit(ev, reads, writes)
        self.n_inst += 1
        return ev

    def barrier(self):
        evs = [(e, self.cnt[e], e) for e in self.eng if self.cnt[e] > 0]
        evs += [(i, self.dcnt[i], "dma") for i in range(len(self.dsem)) if self.dcnt[i] > 0]
        for e in self.eng:
            for ev in evs:
                if ev[2] != e:
                    self._wait(e, ev)
        self.lastw.clear()
        self.reads.clear()

    def close(self):
        self.base.close()


def make_consts():
    c = {}
    c["ident"] = np.eye(128, dtype=np.float32)
    k = np.arange(128)[:, None]
    q = np.arange(128)[None, :]
    c["tri_u"] = (k <= q).astype(np.float32)
    c["ones"] = np.ones((128, 128), np.float32)
    c["fox_mask"] = np.where(k <= q, 0.0, NEG).astype(np.float32)
    slopes = 2.0 ** (-8.0 * (np.arange(8) + 1) / 8)
    B = np.zeros((128, 8, NDEL, 128), np.float32)
    for dl in range(NDEL):
        dist = 128 * dl + q - k
        cnt = ((dist >= 0) & (dist <= 128)).astype(np.float64)
        cnt += ((dist >= 0) & (dist % 4 == 0) & (dist <= 512))
        cnt += ((dist >= 0) & (dist % 16 == 0) & (dist <= 2048))
        for h in range(8):
            with np.errstate(divide="ignore"):
                b = np.where(cnt > 0, np.log(np.maximum(cnt, 1)) - slopes[h] * dist, NEG)
            B[:, h, dl, :] = np.maximum(b, NEG)
    c["dil_bias"] = B
    cc = np.arange(128)[:, None]
    jj = np.arange(128)[None, :]
    c["gdn_m1"] = np.where(jj > cc, 0.0, NEG).astype(np.float32)
    c["gdn_m2"] = np.where(cc >= jj, 0.0, NEG).astype(np.float32)
    return c


CONST_SHAPES = {"ident": [128, 128], "tri_u": [128, 128], "ones": [128, 128],
                "fox_mask": [128, 128], "dil_bias": [128, 8, NDEL, 128],
                "gdn_m1": [128, 128], "gdn_m2": [128, 128]}

IN_SHAPES = {
    "x": [SEQ, D], "attn_w_in": [D, 3080], "fox_forget_bias": [8], "attn_w_out": [D, D],
    "ln_attn_g": [D], "ln_attn_b": [D], "ffn_w_gate": [D, DFF], "ffn_w_up": [D, DFF],
    "ffn_w_down": [DFF, D], "ln_ffn_g": [D], "ln_ffn_b": [D],
    "gdn_w_in": [D, 4112], "gdn_conv_w": [4, 3072], "gdn_a_log": [8], "gdn_dt_bias": [8],
    "gdn_norm_g": [128], "gdn_w_out": [D, D], "ln_gdn_g": [D], "ln_gdn_b": [D],
    "moe_router": [D, NE], "moe_w_gate": [NE, D, DFF], "moe_w_up": [NE, D, DFF],
    "moe_w_down": [NE, DFF, D], "ln_moe_g": [D], "ln_moe_b": [D],
}


class Ctx:
    pass


def emit_xT(C, t, src):
    S, nc = C.S, C.nc
    b = C.xt_i % 2
    C.xt_i += 1
    xb = C.xbf[b]
    S.op("act", [src[1]], [("xbf", b)], nc.scalar.copy, xb[:], src[0])
    for g in range(2):
        pb = (2 * b + g) % 2
        pt = C.ps_tr[pb]
        for j in range(4):
            kc = g * 4 + j
            S.op("pe", [("xbf", b), "identb"], [("pstr", pb)], nc.tensor.transpose,
                 pt[:, j * 128:(j + 1) * 128], xb[:, kc * 128:(kc + 1) * 128], C.identb[:])
        S.op("dve", [("pstr", pb)], [("xT", t)], nc.vector.tensor_copy,
             C.xT[:, g * 4:(g + 1) * 4, t * 128:(t + 1) * 128],
             pt[:].rearrange("p (j c) -> p j c", j=4))


def load_bcast(C, name, vec, n):
    t = C.S.sbuf(name, [128, n], F32)
    C.S.dma([], [name], t[:], vec.partition_broadcast(128))
    return t


def ln_finish(C, t, z, zres, gB, bB, out_d, out_key, make_xT=True, router=None):
    S, nc = C.S, C.nc
    i = C.ln_i % 3
    C.ln_i += 1
    st, mv, sd = C.ln_st[i], C.ln_mv[i], C.ln_sd[i]
    for hf in range(2):
        S.op("dve", [zres], [("lnst", i)], nc.vector.bn_stats, st[:, hf, :], z[:, hf * 512:(hf + 1) * 512])
    S.op("dve", [("lnst", i)], [("lnmv", i)], nc.vector.bn_aggr, mv[:], st[:].rearrange("p a b -> p (a b)"))
    S.op("act", [("lnmv", i)], [("lnsd", i)], nc.scalar.activation, sd[:, 0:1], mv[:, 1:2], AF.Sqrt,
         bias=C.eps_ln[:, 0:1], scale=1.0)
    S.op("dve", [("lnsd", i)], [("lnrs", i)], nc.vector.reciprocal, sd[:, 1:2], sd[:, 0:1])
    zn = C.ln_zn[i]
    S.op("dve", [zres, ("lnmv", i), ("lnrs", i)], [("lnzn", i)], nc.vector.tensor_scalar,
         zn[:], z[:], mv[:, 0:1], sd[:, 1:2], ALU.subtract, ALU.mult)
    ln_flush(C)
    S.op("pool", [("lnzn", i), gB[1]], [("lnzn", i)], nc.gpsimd.tensor_tensor, zn[:], zn[:], gB[0][:], ALU.mult)
    S.op("pool", [("lnzn", i), bB[1]], [("lnzn", i)], nc.gpsimd.tensor_tensor, zn[:], zn[:], bB[0][:], ALU.add)
    S.dma([("lnzn", i)], [(out_key, t)], out_d[t * 128:(t + 1) * 128, :], zn[:], q="pool")
    def later(t=t, zn=zn, i=i):
        if make_xT:
            emit_xT(C, t, (zn[:], ("lnzn", i)))
        if router is not None:
            router(C, t, zn, ("lnzn", i))
    C.ln_pending.append(later)


def ln_flush(C):
    while C.ln_pending:
        C.ln_pending.pop(0)()


def ffn_phase1(C, wg_d, wu_d, hT_d, hkey):
    S, nc = C.S, C.nc
    for cg in range(NFC // 2):
        b = C.w1_i % 2
        C.w1_i += 1
        wg, wu = C.wg[b], C.wu[b]
        S.dma([], [("wg", b)], wg[:], wg_d[:, cg * 256:(cg + 1) * 256].rearrange("(kc p) n -> p kc n", p=128), q="pool")
        S.dma([], [("wu", b)], wu[:], wu_d[:, cg * 256:(cg + 1) * 256].rearrange("(kc p) n -> p kc n", p=128), q="pool")
        for fl in range(2):
            fc = cg * 2 + fl
            hb_i = C.hb_i % 2
            C.hb_i += 1
            hb = C.hbuf[hb_i]
            for tb in range(8):
                pi = C.p1_i % 2
                C.p1_i += 1
                pg, pu = C.ps_g[pi], C.ps_u[pi]
                for kc in range(KC):
                    S.op("pe", [("wg", b), ("xTall",)], [("psg", pi)], nc.tensor.matmul, pg[:],
                         wg[:, kc, fl * 128:(fl + 1) * 128], C.xT[:, kc, tb * 512:(tb + 1) * 512],
                         start=(kc == 0), stop=(kc == KC - 1))
                for kc in range(KC):
                    S.op("pe", [("wu", b), ("xTall",)], [("psu", pi)], nc.tensor.matmul, pu[:],
                         wu[:, kc, fl * 128:(fl + 1) * 128], C.xT[:, kc, tb * 512:(tb + 1) * 512],
                         start=(kc == 0), stop=(kc == KC - 1))
                sg = C.sg[pi]
                S.op("act", [("psg", pi)], [("sg", pi)], nc.scalar.activation, sg[:], pg[:], AF.Silu)
                S.op("dve", [("sg", pi), ("psu", pi)], [("hbuf", hb_i)], nc.vector.tensor_tensor,
                     hb[:, tb * 512:(tb + 1) * 512], sg[:], pu[:], ALU.mult)
            S.dma([("hbuf", hb_i)], [(hkey, fc)], hT_d[:, :, fc, :].rearrange("tb p k -> p tb k"),
                  hb[:].rearrange("p (tb k) -> p tb k", k=256))


def ffn_phase2(C, wd_d, hT_d, hkey, yacc_d, gate, first):
    S, nc = C.S, C.nc
    wd = C.wd
    for q4 in range(4):
        S.dma([], [("wd", q4)], wd[:, q4 * 7:(q4 + 1) * 7, :],
              wd_d[q4 * 896:(q4 + 1) * 896, :].rearrange("(fc p) n -> p fc n", p=128), q="pool")
    for tb in range(16):
        hi = C.hk_i % 2
        C.hk_i += 1
        hk = C.hblk[hi]
        S.dma([(hkey, fc) for fc in range(NFC)], [("hblk", hi)], hk[:], hT_d[tb])
        for j in range(2):
            t = tb * 2 + j
            yi = C.y_i % 2
            C.y_i += 1
            ysb = C.ysb[yi]
            if gate is not None and not first:
                S.dma([("yacc", t)], [("ysb", yi)], ysb[:], yacc_d[t * 128:(t + 1) * 128, :], q="act")
            for hf in range(2):
                pi = C.p2_i % 2
                C.p2_i += 1
                py = C.ps_y[pi]
                for fc in range(NFC):
                    S.op("pe", [("hblk", hi), ("wd", fc // 7)], [("psy", pi)], nc.tensor.matmul, py[:],
                         hk[:, fc, j * 128:(j + 1) * 128], wd[:, fc, hf * 512:(hf + 1) * 512],
                         start=(fc == 0), stop=(fc == NFC - 1))
                ys = ysb[:, hf * 512:(hf + 1) * 512]
                if gate is None:
                    S.op("act", [("psy", pi)], [("ysb", yi)], nc.scalar.copy, ys, py[:])
                elif first:
                    S.op("act", [("psy", pi), "gate"], [("ysb", yi)], nc.scalar.activation, ys, py[:],
                         AF.Copy, scale=gate[0][:, t, gate[1]:gate[1] + 1])
                else:
                    S.op("dve", [("psy", pi), "gate", ("ysb", yi)], [("ysb", yi)], nc.vector.scalar_tensor_tensor,
                         ys, py[:], gate[0][:, t, gate[1]:gate[1] + 1], ys, ALU.mult, ALU.add)
            S.dma([("ysb", yi)], [("yacc", t)], yacc_d[t * 128:(t + 1) * 128, :], ysb[:], q="act")


def ln_from_dram(C, xres_d, xres_key, yacc_d, g_d, b_d, out_d, out_key, make_xT=True, router=None):
    S, nc = C.S, C.nc
    gB = (load_bcast(C, "gB" + out_key, g_d, D), "gB" + out_key)
    bB = (load_bcast(C, "bB" + out_key, b_d, D), "bB" + out_key)
    for t in range(NT):
        i = C.lz_i % 2
        C.lz_i += 1
        xr, yy = C.lnx[i], C.lny[i]
        S.dma([(xres_key, t)], [("lnx", i)], xr[:], xres_d[t * 128:(t + 1) * 128, :])
        S.dma([("yacc", t)], [("lny", i)], yy[:], yacc_d[t * 128:(t + 1) * 128, :])
        S.op("dve", [("lnx", i), ("lny", i)], [("lny", i)], nc.vector.scalar_tensor_tensor,
             yy[:], xr[:], ALPHA, yy[:], ALU.mult, ALU.add)
        ln_finish(C, t, yy, ("lny", i), gB, bB, out_d, out_key, make_xT=make_xT, router=router)
    ln_flush(C)


def alloc_common(C):
    S, nc = C.S, C.nc
    C.xT = S.sbuf("xT", [128, KC, SEQ], BF16)
    C.ident = S.sbuf("ident", [128, 128], F32)
    C.identb = S.sbuf("identb", [128, 128], BF16)
    C.eps_ln = S.sbuf("epsln", [128, 2], F32)
    S.dma([], ["ident"], C.ident[:], C.cd["ident"])
    S.op("dve", ["ident"], ["identb"], nc.vector.tensor_copy, C.identb[:], C.ident[:])
    S.op("pool", [], ["epsln"], nc.gpsimd.memset, C.eps_ln[:, 0:1], LN_EPS)
    S.op("pool", ["epsln"], ["epsln"], nc.gpsimd.memset, C.eps_ln[:, 1:2], RMS_EPS)
    C.xbf = [S.sbuf("xbf", [128, D], BF16) for _ in range(2)]
    C.gate = S.sbuf("gate", [128, NT, NE], F32)
    C.ln_pending = []
    C.xt_i = C.ln_i = C.lz_i = 0
    C.w1_i = C.hb_i = C.p1_i = C.hk_i = C.y_i = C.p2_i = 0


def alloc_ln(C):
    S = C.S
    C.ln_st = [S.sbuf("lnst", [128, 2, 6], F32) for _ in range(3)]
    C.ln_mv = [S.sbuf("lnmv", [128, 2], F32) for _ in range(3)]
    C.ln_sd = [S.sbuf("lnsd", [128, 2], F32) for _ in range(3)]
    C.ln_zn = [S.sbuf("lnzn", [128, D], F32) for _ in range(3)]
    C.lnx = [S.sbuf("lnx", [128, D], F32) for _ in range(2)]
    C.lny = [S.sbuf("lny", [128, D], F32) for _ in range(2)]
    C.ps_tr = [S.psum("pstr", [128, 512], BF16) for _ in range(2)]


def stage_load_xT(C, x_d, key):
    S = C.S
    with S.scope():
        alloc_ln(C)
        for t in range(NT):
            i = t % 2
            S.dma([(key, t)], [("lnx", i)], C.lnx[i][:], x_d[t * 128:(t + 1) * 128, :])
            emit_xT(C, t, (C.lnx[i][:], ("lnx", i)))


def stage_ffn(C, experts, xres_d, xres_key, g_d, b_d, out_d, out_key, gate_sb=None, make_xT=True, router=None):
    S, nc = C.S, C.nc
    with S.scope():
        C.wg = [S.sbuf("wg", [128, KC, 256], BF16) for _ in range(2)]
        C.wu = [S.sbuf("wu", [128, KC, 256], BF16) for _ in range(2)]
        C.hbuf = [S.sbuf("hbuf", [128, SEQ], BF16) for _ in range(2)]
        C.sg = [S.sbuf("sg", [128, 512], BF16) for _ in range(2)]
        C.wd = S.sbuf("wd", [128, NFC, D], BF16)
        C.hblk = [S.sbuf("hblk", [128, NFC, 256], BF16) for _ in range(2)]
        C.ysb = [S.sbuf("ysb", [128, D], F32) for _ in range(2)]
        C.ps_g = [S.psum("psg", [128, 512]) for _ in range(2)]
        C.ps_u = [S.psum("psu", [128, 512]) for _ in range(2)]
        C.ps_y = [S.psum("psy", [128, 512]) for _ in range(2)]
        for e, (wg_d, wu_d, wd_d) in enumerate(experts):
            hT_d = C.hT_d[e % 2]
            hkey = "hT%d" % (e % 2)
            ffn_phase1(C, wg_d, wu_d, hT_d, hkey)
            ffn_phase2(C, wd_d, hT_d, hkey, C.yacc_d, None if gate_sb is None else (gate_sb, e), e == 0)
    with S.scope():
        alloc_ln(C)
        ln_from_dram(C, xres_d, xres_key, C.yacc_d, g_d, b_d, out_d, out_key, make_xT=make_xT, router=router)


def stage_outproj_ln(C, w_d, xres_d, xres_key, g_d, b_d, out_d, out_key, router=None):
    S, nc = C.S, C.nc
    with S.scope():
        alloc_ln(C)
        if router is not None:
            alloc_router(C)
        wout = S.sbuf("wout", [128, KC, D], BF16)
        S.dma([], ["wout"], wout[:], w_d.rearrange("(kc p) n -> p kc n", p=128), q="pool")
        gB = (load_bcast(C, "gB" + out_key, g_d, D), "gB" + out_key)
        bB = (load_bcast(C, "bB" + out_key, b_d, D), "bB" + out_key)
        ps_y = [S.psum("psyo", [128, 512]) for _ in range(3)]
        for t in range(NT):
            i = C.lz_i % 2
            C.lz_i += 1
            xr, yy = C.lnx[i], C.lny[i]
            S.dma([(xres_key, t)], [("lnx", i)], xr[:], xres_d[t * 128:(t + 1) * 128, :])
            for hf in range(2):
                pi = (2 * t + hf) % 3
                for kc in range(KC):
                    S.op("pe", ["wout", ("oTall",)], [("psyo", pi)], nc.tensor.matmul, ps_y[pi][:],
                         C.oT[:, kc, t * 128:(t + 1) * 128], wout[:, kc, hf * 512:(hf + 1) * 512],
                         start=(kc == 0), stop=(kc == KC - 1))
                S.op("dve", [("lnx", i), ("psyo", pi)], [("lny", i)], nc.vector.scalar_tensor_tensor,
                     yy[:, hf * 512:(hf + 1) * 512], xr[:, hf * 512:(hf + 1) * 512], ALPHA, ps_y[pi][:],
                     ALU.mult, ALU.add)
            ln_finish(C, t, yy, ("lny", i), gB, bB, out_d, out_key, router=router)
        ln_flush(C)


def stage_attn(C):
    S, nc, ind = C.S, C.nc, C.ind
    w_in = ind["attn_w_in"]
    KA = [S.sbuf("KA", [128, SEQ], BF16) for _ in range(2)]
    QA = [S.sbuf("QA", [128, SEQ], BF16) for _ in range(2)]
    for hh in range(2):
        S.op("pool", [], [("KA", hh)], nc.gpsimd.memset, KA[hh][:], 0.0)
        S.op("pool", [], [("QA", hh)], nc.gpsimd.memset, QA[hh][:], 0.0)
    BR = [slice(64, 70), slice(0, 6)]
    FR = [slice(0, 64), slice(64, 128)]
    vsb = S.sbuf("vsb", [128, NT, 2, 65], BF16)
    wq2 = [S.sbuf("wq", [128, KC, 128], BF16) for _ in range(2)]
    wk2 = [S.sbuf("wk", [128, KC, 128], BF16) for _ in range(2)]
    wv2 = [S.sbuf("wv", [128, KC, 128], BF16) for _ in range(2)]

    def load_unit_w(n):
        kd, hq = n // 4, n % 4
        b0 = 0 if kd == 0 else 1544
        for (wt2, off, nm) in ((wq2, 0, "wq"), (wk2, 512, "wk"), (wv2, 1024, "wv")):
            c0 = b0 + off + hq * 128
            S.dma([], [(nm, n % 2)], wt2[n % 2][:], w_in[:, c0:c0 + 128].rearrange("(kc p) n -> p kc n", p=128), q="pool")
    wf = S.sbuf("wf", [128, KC, 8], BF16)
    maskb = S.sbuf("maskb", [128, 128], BF16)
    dilB = S.sbuf("dilB", [128, 2, NDEL, 128], BF16)
    fbB = load_bcast(C, "fbB", ind["fox_forget_bias"], 8)
    onecol = S.sbuf("onecol", [128, 1], F32)
    triu = S.sbuf("triu", [128, 128], F32)
    ones = S.sbuf("ones", [128, 128], F32)
    lz = S.sbuf("lz", [128, NT, 8], F32)
    Lc = S.sbuf("Lc", [128, NT, 8], F32)
    tot = S.sbuf("tot", [128, NT, 8], F32)
    pend = S.sbuf("pend", [128, NT, 8], F32)
    pT = [S.sbuf("pT", [128, 512], BF16) for _ in range(3)]
    obf = [S.sbuf("obf", [128, 128], BF16) for _ in range(8)]
    pTall = S.sbuf("pTall", [128, NT, 512], BF16)

    rden = [S.sbuf("rden", [128, 1], F32) for _ in range(2)]
    ps_s = [S.psum("pss", [128, 512]) for _ in range(2)]
    ps_o = [S.psum("pso", [128, 512]) for _ in range(2)]
    ps_d = S.psum("psd", [128, 512])
    selb = S.sbuf("selb", [65, 64], F32)
    S.op("pool", [], ["selb"], nc.gpsimd.memset, selb[:], 0.0)
    S.op("pool", ["selb"], ["selb"], nc.gpsimd.memset, selb[64:65, :], 1.0)
    osb = [S.sbuf("osb", [65, 512], F32) for _ in range(2)]
    rdn = [S.sbuf("rdn", [64, 512], F32) for _ in range(2)]
    obT = [S.sbuf("obT", [64, 512], BF16) for _ in range(2)]

    def finalize(po, po_i, u, hh, G):
        S.op("act", [("pso", po_i)], [("osb", po_i)], nc.scalar.copy, osb[po_i][:], po[0:65, :])
        S.op("pe", [("osb", po_i), "selb"], ["psd"], nc.tensor.matmul, ps_d[0:64, :], selb[:], osb[po_i][:], start=True, stop=True)
        S.op("dve", ["psd"], [("rdn", po_i)], nc.vector.reciprocal, rdn[po_i][:], ps_d[0:64, :])
        S.op("dve", [("osb", po_i), ("rdn", po_i)], [("obT", po_i)], nc.vector.tensor_tensor, obT[po_i][:], osb[po_i][0:64, :], rdn[po_i][:], ALU.mult)
        r0 = u * 128 + hh * 64
        S.dma([("obT", po_i)], [("oT_d", u)], C.oT_d[r0:r0 + 64, G * 512:(G + 1) * 512], obT[po_i][:])

    ps_p = [S.psum("psp", [128, 512]) for _ in range(2)]

    S.dma([], ["maskb"], maskb[:], C.cd["fox_mask"], q="pool")
    S.dma([], ["triu"], triu[:], C.cd["tri_u"])
    S.dma([], ["ones"], ones[:], C.cd["ones"])
    S.op("pool", [], ["onecol"], nc.gpsimd.memset, onecol[:], 1.0)
    S.op("pool", [], ["vsb"], nc.gpsimd.memset, vsb[:, :, :, 64:65], 1.0)
    S.dma([], ["wf"], wf[:], w_in[:, 1536:1544].rearrange("(kc p) n -> p kc n", p=128), q="pool")

    for t in range(NT):
        pi = t % 2
        for kc in range(KC):
            S.op("pe", ["wf"], [("psp", pi)], nc.tensor.matmul, ps_p[pi][:, 0:8],
                 C.xT[:, kc, t * 128:(t + 1) * 128], wf[:, kc, :], start=(kc == 0), stop=(kc == KC - 1))
        S.op("dve", [("psp", pi), "fbB"], ["lz"], nc.vector.tensor_tensor, lz[:, t, :], ps_p[pi][:, 0:8], fbB[:], ALU.add)
    lzf = lz[:].rearrange("p a b -> p (a b)")
    S.op("act", ["lz"], ["lz"], nc.scalar.activation, lzf, lzf, AF.Exp, scale=-1.0)
    S.op("act", ["lz", "onecol"], ["lz"], nc.scalar.activation, lzf, lzf, AF.Ln, bias=onecol[:, 0:1], scale=1.0)
    S.op("pe", ["lz", "triu"], [("psp", 0)], nc.tensor.matmul, ps_p[0][:, 0:256], triu[:], lzf, start=True, stop=True)
    S.op("pe", ["lz", "ones"], [("psp", 1)], nc.tensor.matmul, ps_p[1][:, 0:256], ones[:], lzf, start=True, stop=True)
    S.op("dve", [("psp", 1)], ["tot"], nc.vector.tensor_copy, tot[:].rearrange("p a b -> p (a b)"), ps_p[1][:, 0:256])
    S.op("dve", ["tot"], ["pend"], nc.vector.tensor_copy, pend[:, 0, :], tot[:, 0, :])
    for t in range(1, NT):
        S.op("dve", ["tot", "pend"], ["pend"], nc.vector.tensor_tensor, pend[:, t, :], pend[:, t - 1, :], tot[:, t, :], ALU.add)
    S.op("dve", ["pend", "tot"], ["tot"], nc.vector.tensor_tensor, tot[:], pend[:], tot[:], ALU.subtract)
    S.op("dve", [("psp", 0), "tot"], ["Lc"], nc.vector.tensor_tensor, Lc[:].rearrange("p a b -> p (a b)"),
         ps_p[0][:, 0:256], tot[:].rearrange("p a b -> p (a b)"), ALU.add)

    npend = S.sbuf("npend", [128, NT, 8], F32)
    S.op("dve", ["pend"], ["npend"], nc.vector.tensor_scalar, npend[:].rearrange("p a b -> p (a b)"),
         pend[:].rearrange("p a b -> p (a b)"), -1.0, None, ALU.mult)
    ones3 = S.sbuf("ones3", [3, SEQ], BF16)
    S.op("pool", [], ["ones3"], nc.gpsimd.memset, ones3[:], 1.0)
    for h in range(8):
        S.dma(["ones3"], [("lrow", h)], C.lrow_d[h, 0, 3:6], ones3[:])
        S.dma(["ones3"], [("lrow", h)], C.lrow_d[h, 1, 0:3], ones3[:])
    lsp = [S.sbuf("lsp", [32, 5, 128], F32) for _ in range(2)]
    lsb = [S.sbuf("lsb", [32, 3, 128], BF16) for _ in range(2)]
    ii = 0
    for h in range(8):
        for which, src in ((0, Lc), (1, npend)):
            i = ii % 2
            ii += 1
            pp = ps_p[i]
            S.op("pe", ["Lc", "npend", "ident"], [("psp", i)], nc.tensor.transpose, pp[0:32, 0:128], src[:, :, h], C.ident[:])
            S.op("dve", [("psp", i)], [("lsb", i)], nc.vector.tensor_copy, lsb[i][:, 0, :], pp[0:32, 0:128])
            S.op("dve", [("psp", i), ("lsb", i)], [("lsp", i)], nc.vector.tensor_tensor, lsp[i][:, 0, :], pp[0:32, 0:128], lsb[i][:, 0, :], ALU.subtract)
            S.op("dve", [("lsp", i)], [("lsb", i)], nc.vector.tensor_copy, lsb[i][:, 1, :], lsp[i][:, 0, :])
            S.op("dve", [("lsp", i), ("lsb", i)], [("lsp", i)], nc.vector.tensor_tensor, lsp[i][:, 1, :], lsp[i][:, 0, :], lsb[i][:, 1, :], ALU.subtract)
            S.op("dve", [("lsp", i)], [("lsb", i)], nc.vector.tensor_copy, lsb[i][:, 2, :], lsp[i][:, 1, :])
            S.dma([("lsb", i)], [("lrow", h)], C.lrow_d[h, which, which * 3:which * 3 + 3].rearrange("c (t p) -> t c p", p=128), lsb[i][:])

    cnt = dict(s=0, o=0, p=0, pt=0, ob=0, bq=0)
    for kind in range(2):
        base = 0 if kind == 0 else 1544
        for hp in range(4):
            u = kind * 4 + hp
            wpar = u % 2
            wq, wk, wv = wq2[wpar], wk2[wpar], wv2[wpar]
            if u == 0:
                load_unit_w(0)
            if kind == 1:
                S.dma([], ["dilB"], dilB[:], C.cd["dil_bias"][:, hp * 2:hp * 2 + 2, :, :], q="pool")
                if hp == 0:
                    for hh in range(2):
                        zr = slice(64, 128) if hh == 0 else slice(0, 64)
                        S.op("pool", [], [("KA", hh)], nc.gpsimd.memset, KA[hh][zr, :], 0.0)
                        S.op("pool", [], [("QA", hh)], nc.gpsimd.memset, QA[hh][zr, :], 0.0)
            else:
                for hh in range(2):
                    S.dma([("lrow", hp * 2 + hh)], [("KA", hh)], KA[hh][BR[hh], :], C.lrow_d[hp * 2 + hh, 0])
                    S.dma([("lrow", hp * 2 + hh)], [("QA", hh)], QA[hh][BR[hh], :], C.lrow_d[hp * 2 + hh, 1])
            for (wt, nm, dst, dn) in ((wq, ("wq", wpar), QA, "QA"), (wk, ("wk", wpar), KA, "KA")):
                for tb in range(8):
                    pi = cnt["p"] % 2
                    cnt["p"] += 1
                    for kc in range(KC):
                        S.op("pe", [nm], [("psp", pi)], nc.tensor.matmul, ps_p[pi][:],
                             wt[:, kc, :], C.xT[:, kc, tb * 512:(tb + 1) * 512],
                             start=(kc == 0), stop=(kc == KC - 1))
                    for hh in range(2):
                        if dn == "QA":
                            S.op("act", [("psp", pi)], [(dn, hh)], nc.scalar.mul, dst[hh][FR[hh], tb * 512:(tb + 1) * 512],
                                 ps_p[pi][FR[hh], :], 0.125)
                        else:
                            S.op("dve", [("psp", pi)], [(dn, hh)], nc.vector.tensor_copy, dst[hh][FR[hh], tb * 512:(tb + 1) * 512],
                                 ps_p[pi][FR[hh], :])
            for t4 in range(NT // 4):
                pi = cnt["p"] % 2
                cnt["p"] += 1
                for j in range(4):
                    t = t4 * 4 + j
                    for kc in range(KC):
                        S.op("pe", [("wv", wpar)], [("psp", pi)], nc.tensor.matmul, ps_p[pi][:, j * 128:(j + 1) * 128],
                             C.xT[:, kc, t * 128:(t + 1) * 128], wv[:, kc, :],
                             start=(kc == 0), stop=(kc == KC - 1))
                S.op("dve", [("psp", pi)], ["vsb"], nc.vector.tensor_copy, vsb[:, t4 * 4:(t4 + 1) * 4, :, 0:64],
                     ps_p[pi][:].rearrange("p (a b c) -> p a b c", a=4, b=2))
            if u + 1 < 8:
                load_unit_w(u + 1)
            if kind == 0:
                for G in range(NT // 4):
                    gi = cnt["ob"] % 2
                    cnt["ob"] += 1
                    for hh in range(2):
                        h = hp * 2 + hh
                        hb = hh * 64
                        nk = 4 * G + 4
                        for j in range(nk):
                            si = cnt["s"] % 2
                            cnt["s"] += 1
                            ps = ps_s[si]
                            i0 = max(0, j - 4 * G)
                            c0 = i0 * 128
                            S.op("pe", [("KA", hh), ("QA", hh)], [("pss", si)], nc.tensor.matmul, ps[:, c0:512],
                                 KA[hh][:, j * 128:(j + 1) * 128], QA[hh][:, G * 512 + c0:(G + 1) * 512],
                                 start=True, stop=(j < 4 * G))
                            if j >= 4 * G:
                                S.op("pe", ["maskb", "identb"], [("pss", si)], nc.tensor.matmul, ps[:, c0:c0 + 128],
                                     C.identb[:], maskb[:], start=False, stop=True)
                            S.op("act", [("pss", si)], [("pTall", j)], nc.scalar.activation, pTall[:, j, c0:512],
                                 ps[:, c0:512], AF.Exp)
                        po_i = cnt["o"] % 2
                        cnt["o"] += 1
                        po = ps_o[po_i]
                        for j in range(nk):
                            c0 = max(0, j - 4 * G) * 128
                            S.op("pe", [("pTall", j), "vsb"], [("pso", po_i)], nc.tensor.matmul, po[0:65, c0:512],
                                 vsb[:, j, hh, :], pTall[:, j, c0:512], start=(j == 0), stop=(j == nk - 1), skip_group_check=True)
                        finalize(po, po_i, u, hh, G)
                continue
            for G in range(NT // 4):
                gi = cnt["ob"] % 2
                cnt["ob"] += 1
                for hh in range(2):
                    hb = hh * 64
                    jlo = max(0, 4 * G - (NDEL - 1))
                    for j in range(jlo, 4 * G + 4):
                        si = cnt["s"] % 2
                        cnt["s"] += 1
                        ps = ps_s[si]
                        i0 = max(0, j - 4 * G)
                        i1 = min(3, j + (NDEL - 1) - 4 * G)
                        c0, c1 = i0 * 128, (i1 + 1) * 128
                        d0 = 4 * G + i0 - j
                        S.op("pe", [("KA", hh), ("QA", hh)], [("pss", si)], nc.tensor.matmul, ps[:, c0:c1],
                             KA[hh][:, j * 128:(j + 1) * 128], QA[hh][:, G * 512 + c0:G * 512 + c1],
                             start=True, stop=False)
                        S.op("pe", ["dilB", "identb"], [("pss", si)], nc.tensor.matmul, ps[:, c0:c1], C.identb[:],
                             dilB[:, hh, d0:d0 + (i1 - i0 + 1), :].rearrange("p a b -> p (a b)"), start=False, stop=True)
                        S.op("act", [("pss", si)], [("pTall", j - jlo)], nc.scalar.activation, pTall[:, j - jlo, c0:c1],
                             ps[:, c0:c1], AF.Exp)
                    po_i = cnt["o"] % 2
                    cnt["o"] += 1
                    po = ps_o[po_i]
                    js = list(range(jlo, 4 * G + 4))
                    for j in js:
                        i0 = max(0, j - 4 * G)
                        i1 = min(3, j + (NDEL - 1) - 4 * G)
                        c0, c1 = i0 * 128, (i1 + 1) * 128
                        S.op("pe", [("pTall", j - jlo), "vsb"], [("pso", po_i)], nc.tensor.matmul, po[0:65, c0:c1],
                             vsb[:, j, hh, :], pTall[:, j - jlo, c0:c1], start=(j == js[0]), stop=(j == js[-1]), skip_group_check=True)
                    finalize(po, po_i, u, hh, G)


def alloc_router(C):
    S, nc = C.S, C.nc
    C.rt_w = S.sbuf("rtw", [128, KC, NE], F32)
    S.dma([], ["rtw"], C.rt_w[:], C.ind["moe_router"].rearrange("(kc p) n -> p kc n", p=128))
    C.rt_xT = S.sbuf("rtxT", [128, KC, 128], F32)
    C.rt_ps = [S.psum("rtps", [128, 512]) for _ in range(2)]
    C.rt_pl = S.psum("rtpl", [128, 8])
    C.rt_sm = S.sbuf("rtsm", [128, 6, 8], F32)


def router_tile(C, t, zn, znkey):
    S, nc = C.S, C.nc
    for g in range(2):
        for j in range(4):
            kc = g * 4 + j
            S.op("pe", [znkey, "ident"], [("rtps", g)], nc.tensor.transpose,
                 C.rt_ps[g][:, j * 128:(j + 1) * 128], zn[:, kc * 128:(kc + 1) * 128], C.ident[:])
        S.op("act", [("rtps", g)], ["rtxT"], nc.scalar.copy, C.rt_xT[:, g * 4:(g + 1) * 4, :],
             C.rt_ps[g][:].rearrange("p (j c) -> p j c", j=4))
    for kc in range(KC):
        S.op("pe", ["rtxT", "rtw"], ["rtpl"], nc.tensor.matmul, C.rt_pl[:], C.rt_xT[:, kc, :], C.rt_w[:, kc, :],
             start=(kc == 0), stop=(kc == KC - 1))
    sm = C.rt_sm
    lg, srt, msk, ex, nv, den = (sm[:, i, :] for i in range(6))
    S.op("dve", ["rtpl"], ["rtsm"], nc.vector.tensor_copy, lg, C.rt_pl[:])
    S.op("dve", ["rtsm"], ["rtsm"], nc.vector.max, srt, lg)
    S.op("dve", ["rtsm"], ["rtsm"], nc.vector.tensor_scalar, msk, lg, sm[:, 1, 1:2], None, ALU.is_ge)
    S.op("dve", ["rtsm"], ["rtsm"], nc.vector.tensor_scalar, nv[:, 0:1], sm[:, 1, 0:1], -1.0, None, ALU.mult)
    S.op("act", ["rtsm"], ["rtsm"], nc.scalar.activation, ex, lg, AF.Exp, bias=sm[:, 4, 0:1], scale=1.0)
    S.op("dve", ["rtsm"], ["rtsm"], nc.vector.tensor_tensor, ex, ex, msk, ALU.mult)
    S.op("dve", ["rtsm"], ["rtsm"], nc.vector.reduce_sum, den[:, 0:1], ex, mybir.AxisListType.X)
    S.op("dve", ["rtsm"], ["rtsm"], nc.vector.reciprocal, den[:, 1:2], den[:, 0:1])
    S.op("dve", ["rtsm"], ["gate"], nc.vector.tensor_scalar, C.gate[:, t, :], ex, sm[:, 5, 1:2], None, ALU.mult)


def stage_gdn(C):
    import os
    S, nc, ind = C.S, C.nc, C.ind
    w_in = ind["gdn_w_in"]
    X = mybir.AxisListType.X
    sb = lambda n, shp, dt=F32: S.sbuf(n, shp, dt)
    onecol = sb("onecol", [128, 1]); triu = sb("triu", [128, 128]); ones = sb("ones", [128, 128])
    nm1 = sb("nm1", [128, 128]); nm2 = sb("nm2", [128, 128])
    S.dma([], ["triu"], triu[:], C.cd["tri_u"]); S.dma([], ["ones"], ones[:], C.cd["ones"])
    S.dma([], ["nm1"], nm1[:], C.cd["gdn_m1"]); S.dma([], ["nm2"], nm2[:], C.cd["gdn_m2"])
    S.op("pool", [], ["onecol"], nc.gpsimd.memset, onecol[:], 1.0)
    dtbB = load_bcast(C, "dtbB", ind["gdn_dt_bias"], 8)
    alogB = load_bcast(C, "alogB", ind["gdn_a_log"], 8)
    ngB = load_bcast(C, "ngB", ind["gdn_norm_g"], 128)
    wba = sb("wba", [128, KC, 16], BF16)
    S.dma([], ["wba"], wba[:], w_in[:, 3072:3088].rearrange("(kc p) n -> p kc n", p=128), q="pool")
    cwr = sb("cwr", [96, 128]); cw = sb("cw", [128, 96])
    S.dma([], ["cwr"], cwr[:], ind["gdn_conv_w"].rearrange("j (c p) -> (j c) p", p=128))
    P = [S.psum("gp", [128, 512]) for _ in range(7)]
    TBM = S.psum("gtb", [128, 512], BF16)
    TBs = [TBM, TBM]
    pk = lambda i: ("gp", i)
    S.op("pe", ["cwr", "ident"], [pk(0)], nc.tensor.transpose, P[0][:, 0:96], cwr[:], C.ident[0:96, 0:96])
    S.op("dve", [pk(0)], ["cw"], nc.vector.tensor_copy, cw[:], P[0][:, 0:96])
    names = ["braw", "araw", "beta", "gneg", "Gc", "Gl", "egam", "egl", "gtot"]
    sc = {n: sb(n, [128, NT, 8]) for n in names}
    fl = lambda n: sc[n][:].rearrange("p a b -> p (a b)")
    S.op("act", ["alogB"], ["alogB"], nc.scalar.activation, alogB[:], alogB[:], AF.Exp)
    for t in range(NT):
        pi = t % 2
        for kc in range(KC):
            S.op("pe", ["wba"], [pk(pi)], nc.tensor.matmul, P[pi][:, 0:16], C.xT[:, kc, t * 128:(t + 1) * 128],
                 wba[:, kc, :], start=(kc == 0), stop=(kc == KC - 1))
        S.op("dve", [pk(pi)], ["braw"], nc.vector.tensor_copy, sc["braw"][:, t, :], P[pi][:, 0:8])
        S.op("dve", [pk(pi), "dtbB"], ["araw"], nc.vector.tensor_tensor, sc["araw"][:, t, :], P[pi][:, 8:16], dtbB[:], ALU.add)
        S.op("dve", ["araw"], ["araw"], nc.vector.tensor_copy, sc["araw"][:, t, :], sc["araw"][:, t, :]) if False else None
    S.op("act", ["braw"], ["braw"], nc.scalar.activation, fl("braw"), fl("braw"), AF.Exp, scale=-1.0)
    S.op("dve", ["braw"], ["braw"], nc.vector.tensor_scalar, fl("braw"), fl("braw"), 1.0, None, ALU.add)
    S.op("dve", ["braw"], ["beta"], nc.vector.reciprocal, fl("beta"), fl("braw"))
    S.op("act", ["araw"], ["araw"], nc.scalar.activation, fl("araw"), fl("araw"), AF.Exp)
    S.op("act", ["araw", "onecol"], ["araw"], nc.scalar.activation, fl("araw"), fl("araw"), AF.Ln, bias=onecol[:, 0:1], scale=1.0)
    for t in range(NT):
        S.op("dve", ["araw", "alogB"], ["gneg"], nc.vector.tensor_tensor, sc["gneg"][:, t, :], sc["araw"][:, t, :], alogB[:], ALU.mult)
    S.op("pe", ["gneg", "triu"], [pk(0)], nc.tensor.matmul, P[0][:, 0:256], triu[:], fl("gneg"), start=True, stop=True)
    S.op("pe", ["gneg", "ones"], [pk(1)], nc.tensor.matmul, P[1][:, 0:256], ones[:], fl("gneg"), start=True, stop=True)
    S.op("dve", [pk(0)], ["Gc"], nc.vector.tensor_copy, fl("Gc"), P[0][:, 0:256])
    S.op("dve", [pk(1)], ["Gl"], nc.vector.tensor_copy, fl("Gl"), P[1][:, 0:256])
    S.op("act", ["Gc"], ["egam"], nc.scalar.activation, fl("egam"), fl("Gc"), AF.Exp, scale=-1.0)
    S.op("act", ["Gl"], ["gtot"], nc.scalar.activation, fl("gtot"), fl("Gl"), AF.Exp, scale=-1.0)
    S.op("dve", ["Gc", "Gl"], ["egl"], nc.vector.tensor_tensor, fl("egl"), fl("Gc"), fl("Gl"), ALU.subtract)
    S.op("act", ["egl"], ["egl"], nc.scalar.activation, fl("egl"), fl("egl"), AF.Exp)

    wqkv2 = [[sb("wqkv", [128, KC, 128], BF16) for _ in range(3)] for _ in range(2)]
    wgt2 = [sb("wgt", [128, KC, 128], BF16) for _ in range(2)]

    def load_head_w(hd):
        pr = hd % 2
        for ci in range(3):
            col = ci * 1024 + hd * 128
            S.dma([], [("wqkv", pr, ci)], wqkv2[pr][ci][:], w_in[:, col:col + 128].rearrange("(kc p) n -> p kc n", p=128), q="pool")
        S.dma([], [("wgt", pr)], wgt2[pr][:], w_in[:, 3088 + hd * 128:3088 + (hd + 1) * 128].rearrange("(kc p) n -> p kc n", p=128), q="pool")
    hpre = sb("hpre", [128, SEQ + 4], BF16)
    fT = [sb("fT", [128, SEQ], BF16) for _ in range(3)]
    dg = sb("dg", [128, 12, 128], BF16)
    ktm = sb("ktm", [128, NT, 128], BF16); vb = sb("vb", [128, NT, 128], BF16)
    kbg = sb("kbg", [128, NT, 128], BF16); kdec = ktm
    qkT = sb("qkT", [128, NT, 128], BF16)
    usb = sb("usb", [128, NT, 128], BF16)
    hs = {n: sb("hs_" + n, [128, NT]) for n in ["ssqk", "ssqq", "lnk", "lnq", "rk", "rq", "r1", "r2", "nGc",
                                                 "skbg", "skdec", "sqg", "nbrk"]}
    junk = sb("junk", [128, 128])
    junk2 = sb("junk2", [128, 128])
    qtm = [sb("qtm", [128, 128], BF16) for _ in range(2)]
    d12 = [sb("d12", [128, 2, 128]) for _ in range(2)]
    E12 = [sb("E12", [128, 2, 128]) for _ in range(2)]
    Mb = [[[sb("Mb", [128, 128], BF16) for _ in range(2)] for _ in range(4)] for _ in range(2)]
    Mtb = [[[sb("Mtb", [128, 128], BF16) for _ in range(2)] for _ in range(4)] for _ in range(2)]
    Yb = [[[sb("Yb", [128, 128], BF16) for _ in range(2)] for _ in range(4)] for _ in range(2)]
    Sf = sb("Sf", [128, 128]); Sb = sb("Sb", [128, 128], BF16)
    vnew = [sb("vnew", [128, 128], BF16) for _ in range(2)]
    o1 = [sb("o1", [128, 128]) for _ in range(2)]
    sgall = sb("sgall", [128, NT, 128], BF16)
    og = [sb("og", [128, 128], BF16) for _ in range(2)]
    osm = [sb("osm", [128, 4]) for _ in range(2)]
    S.op("pool", [], ["hpre"], nc.gpsimd.memset, hpre[:, 0:4], 0.0)
    wT = fT[2][:].rearrange("p (a b) -> p a b", a=NT)

    import os
    STOP = os.environ.get("GDN_STOP", "")
    if STOP == "A":
        return
    for h in range(8 if not STOP else 1):
        Gc_h, beta_h = sc["Gc"][:, :, h], sc["beta"][:, :, h]
        egam_h, egl_h = sc["egam"][:, :, h], sc["egl"][:, :, h]
        hpar = h % 2
        wqkv, wgt = wqkv2[hpar], wgt2[hpar]
        if h == 0:
            load_head_w(0)
        for ci in range(3):
            for j in range(4):
                idx = j * 24 + ci * 8 + h
                S.op("dve", ["cw", "ident"], ["dg"], nc.vector.tensor_scalar, dg[:, ci * 4 + j, :], C.ident[:],
                     cw[:, idx:idx + 1], None, ALU.mult)
        for ci in range(3):
            for tb in range(8):
                pi = tb % 2
                for kc in range(KC):
                    S.op("pe", [("wqkv", hpar, ci)], [pk(pi)], nc.tensor.matmul, P[pi][:], wqkv[ci][:, kc, :],
                         C.xT[:, kc, tb * 512:(tb + 1) * 512], start=(kc == 0), stop=(kc == KC - 1))
                S.op("dve", [pk(pi)], ["hpre"], nc.vector.tensor_copy, hpre[:, 4 + tb * 512:4 + (tb + 1) * 512], P[pi][:])
            for tb in range(8):
                pi = 2 + tb % 2
                for j in range(4):
                    S.op("pe", ["hpre", "dg"], [pk(pi)], nc.tensor.matmul, P[pi][:], dg[:, ci * 4 + j, :],
                         hpre[:, 1 + tb * 512 + j:1 + tb * 512 + j + 512], start=(j == 0), stop=(j == 3))
                S.op("act", [pk(pi)], [("fT", ci)], nc.scalar.activation, fT[ci][:, tb * 512:(tb + 1) * 512], P[pi][:], AF.Silu)
        if STOP == "B1":
            return
        if h + 1 < 8 and not STOP:
            load_head_w(h + 1)
        for t in range(NT):
            sl = slice(t * 128, (t + 1) * 128)
            pi = ("tbm",)
            o0 = 0
            TB = TBs[t % 2]
            S.op("pe", [("fT", 0), "identb"], [pi], nc.tensor.transpose, TB[:, o0:o0 + 128], fT[0][:, sl], C.identb[:])
            S.op("pe", [("fT", 1), "identb"], [pi], nc.tensor.transpose, TB[:, o0 + 128:o0 + 256], fT[1][:, sl], C.identb[:])
            S.op("pe", [("fT", 2), "identb"], [pi], nc.tensor.transpose, TB[:, o0 + 256:o0 + 384], fT[2][:, sl], C.identb[:])
            LV = int(os.environ.get("B2LV", "9"))
            if LV >= 1:
                S.op("dve", [pi], [("qtm", t % 2)], nc.vector.tensor_copy, qtm[t % 2][:], TB[:, o0:o0 + 128])
                S.op("dve", [pi], ["ktm"], nc.vector.tensor_copy, ktm[:, t, :], TB[:, o0 + 128:o0 + 256])
            if LV >= 2:
                S.op("dve", [pi, "beta"], ["vb"], nc.vector.tensor_scalar, vb[:, t, :], TB[:, o0 + 256:o0 + 384],
                     sc["beta"][:, t, h:h + 1], None, ALU.mult)
            if LV >= 3:
                S.op("act", [("qtm", t % 2)], ["junk"], nc.scalar.activation, junk[:], qtm[t % 2][:], AF.Square)
                S.op("act", ["ktm"], ["junk2"], nc.scalar.activation, junk2[:], ktm[:, t, :], AF.Square)
            if LV >= 4:
                S.op("dve", ["junk"], ["ssqq"], nc.vector.reduce_sum, hs["ssqq"][:, t:t + 1], junk[:], X)
                S.op("dve", ["junk2"], ["ssqk"], nc.vector.reduce_sum, hs["ssqk"][:, t:t + 1], junk2[:], X)
        if STOP == "B2":
            return
        H = lambda n: hs[n][:]
        S.op("act", ["ssqk", "epsln"], ["lnk"], nc.scalar.activation, H("lnk"), H("ssqk"), AF.Ln, bias=C.eps_ln[:, 1:2], scale=1.0)
        S.op("act", ["ssqq", "epsln"], ["lnq"], nc.scalar.activation, H("lnq"), H("ssqq"), AF.Ln, bias=C.eps_ln[:, 1:2], scale=1.0)
        S.op("act", ["lnk"], ["rk"], nc.scalar.activation, H("rk"), H("lnk"), AF.Exp, scale=-0.5)
        S.op("act", ["lnq"], ["rq"], nc.scalar.activation, H("rq"), H("lnq"), AF.Exp, scale=-0.5)
        S.op("dve", ["lnk", "Gc"], ["r1"], nc.vector.scalar_tensor_tensor, H("r1"), H("lnk"), -0.5, Gc_h, ALU.mult, ALU.add)
        S.op("dve", ["lnq", "Gc"], ["r2"], nc.vector.scalar_tensor_tensor, H("r2"), H("lnq"), -0.5, Gc_h, ALU.mult, ALU.subtract)
        S.op("dve", ["r2"], ["r2"], nc.vector.tensor_scalar, H("r2"), H("r2"), -0.5 * math.log(128.0), None, ALU.add)
        S.op("dve", ["Gc"], ["nGc"], nc.vector.tensor_scalar, H("nGc"), Gc_h, -1.0, None, ALU.mult)
        S.op("dve", ["rk", "beta"], ["skbg"], nc.vector.tensor_tensor, H("skbg"), H("rk"), beta_h, ALU.mult)
        S.op("dve", ["skbg", "egam"], ["skbg"], nc.vector.tensor_tensor, H("skbg"), H("skbg"), egam_h, ALU.mult)
        S.op("dve", ["rk", "egl"], ["skdec"], nc.vector.tensor_tensor, H("skdec"), H("rk"), egl_h, ALU.mult)
        S.op("dve", ["rq", "egam"], ["sqg"], nc.vector.scalar_tensor_tensor, H("sqg"), H("rq"), 128.0 ** -0.5, egam_h, ALU.mult, ALU.mult)
        S.op("dve", ["rk", "beta"], ["nbrk"], nc.vector.scalar_tensor_tensor, H("nbrk"), beta_h, -1.0, H("rk"), ALU.mult, ALU.mult)
        for t in range(NT):
            S.op("dve", ["ktm", "skbg"], ["kbg"], nc.vector.tensor_scalar, kbg[:, t, :], ktm[:, t, :], hs["skbg"][:, t:t + 1], None, ALU.mult)
            S.op("dve", ["ktm", "skdec"], ["ktm"], nc.vector.tensor_scalar, kdec[:, t, :], ktm[:, t, :], hs["skdec"][:, t:t + 1], None, ALU.mult)
        if STOP == "B3":
            return
        def b4_setup_tile(t0, par, b4):
            t = t0 + b4
            sl = slice(t * 128, (t + 1) * 128)
            cs = slice(b4 * 128, (b4 + 1) * 128)
            di = t % 2
            S.op("dve", ["r1", "nm1"], [("d12", di)], nc.vector.tensor_scalar, d12[di][:, 0, :], nm1[:], hs["r1"][:, t:t + 1], None, ALU.add)
            S.op("dve", ["r2", "nm2"], [("d12", di)], nc.vector.tensor_scalar, d12[di][:, 1, :], nm2[:], hs["r2"][:, t:t + 1], None, ALU.add)
            S.op("pe", [("fT", 1)], [pk(0)], nc.tensor.matmul, P[0][:, cs], fT[1][:, sl], fT[1][:, sl], start=True, stop=True)
            S.op("pe", [("fT", 1), ("fT", 0)], [pk(1)], nc.tensor.matmul, P[1][:, cs], fT[1][:, sl], fT[0][:, sl], start=True, stop=True)
            S.op("pe", [("d12", di), "ident"], [pk(2)], nc.tensor.transpose, P[2][:, cs], d12[di][:, 0, :], C.ident[:])
            S.op("pe", [("d12", di), "ident"], [pk(3)], nc.tensor.transpose, P[3][:, cs], d12[di][:, 1, :], C.ident[:])
            S.op("act", [pk(2), "nGc"], [("E12", di)], nc.scalar.activation, E12[di][:, 0, :], P[2][:, cs], AF.Exp,
                 bias=hs["nGc"][:, t:t + 1], scale=1.0)
            S.op("act", [pk(3), "Gc"], [("E12", di)], nc.scalar.activation, E12[di][:, 1, :], P[3][:, cs], AF.Exp,
                 bias=sc["Gc"][:, t, h:h + 1], scale=1.0)
            S.op("dve", [pk(0), ("E12", di), "nbrk"], [("Mb", par, b4, 0)], nc.vector.scalar_tensor_tensor, Mb[par][b4][0][:],
                 P[0][:, cs], hs["nbrk"][:, t:t + 1], E12[di][:, 0, :], ALU.mult, ALU.mult)
            S.op("dve", [pk(1), ("E12", di), "rk"], ["qkT"], nc.vector.scalar_tensor_tensor, qkT[:, t, :],
                 P[1][:, cs], hs["rk"][:, t:t + 1], E12[di][:, 1, :], ALU.mult, ALU.mult)
            S.op("pe", [("Mb", par, b4, 0), "identb"], [("tbm",)], nc.tensor.transpose, TBM[:, cs], Mb[par][b4][0][:], C.identb[:])
            S.op("dve", [("tbm",)], [("Mtb", par, b4, 0)], nc.vector.tensor_copy, Mtb[par][b4][0][:], TBM[:, cs])
            S.op("dve", [("Mtb", par, b4, 0), "identb"], [("Yb", par, b4, 0)], nc.vector.tensor_tensor, Yb[par][b4][0][:],
                 Mtb[par][b4][0][:], C.identb[:], ALU.add)

        def b4_level(par, lvl, cur):
            nxt = 1 - cur
            for b4 in range(4):
                cs = slice(b4 * 128, (b4 + 1) * 128)
                S.op("pe", [("Mtb", par, b4, cur), ("Mb", par, b4, cur)], [pk(4)], nc.tensor.matmul, P[4][:, cs],
                     Mtb[par][b4][cur][:], Mb[par][b4][cur][:], start=True, stop=True)
                if lvl < 6:
                    S.op("pe", [("Mtb", par, b4, cur), ("Mb", par, b4, cur)], [pk(5)], nc.tensor.matmul, P[5][:, cs],
                         Mb[par][b4][cur][:], Mtb[par][b4][cur][:], start=True, stop=True)
            for b4 in range(4):
                cs = slice(b4 * 128, (b4 + 1) * 128)
                S.op("act", [pk(4)], [("Mb", par, b4, nxt)], nc.scalar.copy, Mb[par][b4][nxt][:], P[4][:, cs])
                if lvl < 6:
                    S.op("dve", [pk(5)], [("Mtb", par, b4, nxt)], nc.vector.tensor_copy, Mtb[par][b4][nxt][:], P[5][:, cs])
            for b4 in range(4):
                cs = slice(b4 * 128, (b4 + 1) * 128)
                S.op("pe", [("Mb", par, b4, nxt), ("Yb", par, b4, cur)], [pk(6)], nc.tensor.matmul, P[6][:, cs],
                     Mb[par][b4][nxt][:], Yb[par][b4][cur][:], start=True, stop=True)
            for b4 in range(4):
                cs = slice(b4 * 128, (b4 + 1) * 128)
                S.op("dve", [pk(6), ("Yb", par, b4, cur)], [("Yb", par, b4, nxt)], nc.vector.tensor_tensor, Yb[par][b4][nxt][:],
                     Yb[par][b4][cur][:], P[6][:, cs], ALU.add)
            return nxt

        def b4_tail(t0, par, cur):
            for b4 in range(4):
                t = t0 + b4
                cs = slice(b4 * 128, (b4 + 1) * 128)
                S.op("pe", [("Yb", par, b4, cur), "vb"], [pk(4)], nc.tensor.matmul, P[4][:, cs], Yb[par][b4][cur][:], vb[:, t, :], start=True, stop=True)
                S.op("pe", [("Yb", par, b4, cur), "kbg"], [pk(5)], nc.tensor.matmul, P[5][:, cs], kbg[:, t, :], Yb[par][b4][cur][:], start=True, stop=True)
            S.op("act", [pk(4)], ["usb"], nc.scalar.copy, usb[:, t0:t0 + 4, :], P[4][:].rearrange("p (a b) -> p a b", a=4))
            S.op("dve", [pk(5)], [("fT", 2)], nc.vector.tensor_copy, wT[:, t0:t0 + 4, :], P[5][:].rearrange("p (a b) -> p a b", a=4))

        for b4 in range(4):
            b4_setup_tile(0, 0, b4)
        for n in range(NT // 4):
            par = n % 2
            cur = 0
            for lvl in range(1, 7):
                cur = b4_level(par, lvl, cur)
                if n + 1 < NT // 4 and lvl <= 4:
                    b4_setup_tile((n + 1) * 4, 1 - par, lvl - 1)
            b4_tail(n * 4, par, cur)
        if STOP == "B4":
            return
        for t4 in range(NT // 4):
            gp = 4 + t4 % 2
            for j in range(4):
                t = t4 * 4 + j
                for kc in range(KC):
                    S.op("pe", [("wgt", hpar)], [pk(gp)], nc.tensor.matmul, P[gp][:, j * 128:(j + 1) * 128],
                         C.xT[:, kc, t * 128:(t + 1) * 128], wgt[:, kc, :], start=(kc == 0), stop=(kc == KC - 1))
            S.op("act", [pk(gp)], ["sgall"], nc.scalar.activation, sgall[:, t4 * 4:(t4 + 1) * 4, :],
                 P[gp][:].rearrange("p (a b) -> p a b", a=4), AF.Silu)
        S.op("pool", [], ["Sf"], nc.gpsimd.memset, Sf[:], 0.0)
        S.op("pool", [], ["Sb"], nc.gpsimd.memset, Sb[:], 0.0)
        for t in range(NT):
            sl = slice(t * 128, (t + 1) * 128)
            i2 = t % 2
            S.op("pe", [("fT", 2), "Sb"], [pk(2)], nc.tensor.matmul, P[2][:, 0:128], wT[:, t, :], Sb[:], start=True, stop=True)
            S.op("pe", [("fT", 0), "Sb"], [pk(2)], nc.tensor.matmul, P[2][:, 128:256], fT[0][:, sl], Sb[:], start=True, stop=True)
            B5S = int(os.environ.get("B5S", "9"))
            if B5S >= 1:
                S.op("dve", [pk(2), "usb"], [("vnew", i2)], nc.vector.tensor_tensor, vnew[i2][:], usb[:, t, :], P[2][:, 0:128], ALU.subtract)
            if B5S >= 2:
                S.op("dve", [pk(2), "sqg"], [("o1", i2)], nc.vector.tensor_scalar, o1[i2][:], P[2][:, 128:256], hs["sqg"][:, t:t + 1], None, ALU.mult)
            B5 = int(os.environ.get("B5LV", "9"))
            if B5 < 2:
                continue
            S.op("pe", ["qkT", ("vnew", i2)], [pk(3)], nc.tensor.matmul, P[3][:, 0:128], qkT[:, t, :], vnew[i2][:], start=True, stop=True)
            S.op("pe", ["ktm", ("vnew", i2)], [pk(3)], nc.tensor.matmul, P[3][:, 128:256], kdec[:, t, :], vnew[i2][:], start=True, stop=True)
            S.op("dve", [pk(3), ("o1", i2)], [("o1", i2)], nc.vector.tensor_tensor, o1[i2][:], o1[i2][:], P[3][:, 0:128], ALU.add)
            S.op("dve", [pk(3), "Sf", "gtot"], ["Sb"], nc.vector.scalar_tensor_tensor, Sb[:], Sf[:], sc["gtot"][:, t, h:h + 1],
                 P[3][:, 128:256], ALU.mult, ALU.add)
            S.op("dve", [pk(3), "Sf", "gtot"], ["Sf"], nc.vector.scalar_tensor_tensor, Sf[:], Sf[:], sc["gtot"][:, t, h:h + 1],
                 P[3][:, 128:256], ALU.mult, ALU.add)
            if B5 < 3:
                continue
            if B5 < 4:
                continue
            S.op("act", [("o1", i2)], ["junk"], nc.scalar.activation, junk[:], o1[i2][:], AF.Square)
            S.op("dve", ["junk"], [("osm", i2)], nc.vector.reduce_sum, osm[i2][:, 0:1], junk[:], X)
            S.op("act", [("osm", i2), "epsln"], [("osm", i2)], nc.scalar.activation, osm[i2][:, 1:2], osm[i2][:, 0:1], AF.Sqrt,
                 bias=C.eps_ln[:, 1:2], scale=1.0 / 128.0)
            S.op("dve", [("osm", i2)], [("osm", i2)], nc.vector.reciprocal, osm[i2][:, 2:3], osm[i2][:, 1:2])
            S.op("dve", [("o1", i2), ("osm", i2), "ngB"], [("o1", i2)], nc.vector.scalar_tensor_tensor, o1[i2][:], o1[i2][:],
                 osm[i2][:, 2:3], ngB[:], ALU.mult, ALU.mult)
            if B5 < 5:
                continue
            S.op("pool", [("o1", i2), "sgall"], [("og", i2)], nc.gpsimd.tensor_tensor, og[i2][:], o1[i2][:], sgall[:, t, :], ALU.mult)
            S.dma([("og", i2)], [("o_d", t, h)], C.o_d[t * 128:(t + 1) * 128, h * 128:(h + 1) * 128], og[i2][:])


def stage_load_oT(C):
    S, nc = C.S, C.nc
    with S.scope():
        ob = [S.sbuf("ob", [128, D], BF16) for _ in range(2)]
        pst = [S.psum("pstro", [128, 512], BF16) for _ in range(2)]
        for t in range(NT):
            i = t % 2
            S.dma([("o_d", t, h) for h in range(8)] + [("o_d", t)], [("ob", i)], ob[i][:], C.o_d[t * 128:(t + 1) * 128, :])
            for g in range(2):
                for j in range(4):
                    kc = g * 4 + j
                    S.op("pe", [("ob", i), "identb"], [("pstro", g)], nc.tensor.transpose,
                         pst[g][:, j * 128:(j + 1) * 128], ob[i][:, kc * 128:(kc + 1) * 128], C.identb[:])
                S.op("dve", [("pstro", g)], [("oTall",)], nc.vector.tensor_copy,
                     C.oT[:, g * 4:(g + 1) * 4, t * 128:(t + 1) * 128], pst[g][:].rearrange("p (j c) -> p j c", j=4))


def build(stages, dbg_in=None, dbg_out=None):
    nc = bass.Bass("TRN2", target_bir_lowering=False)
    C = Ctx()
    C.nc = nc
    C.S = S = Sched(nc)
    ind = {k: nc.dram_tensor(k, v, F32, kind="ExternalInput").ap() for k, v in IN_SHAPES.items()}
    C.cd = {k: nc.dram_tensor("c_" + k, v, F32, kind="ExternalInput").ap() for k, v in CONST_SHAPES.items()}
    out_d = nc.dram_tensor("out", [SEQ, D], F32, kind="ExternalOutput").ap()
    C.ind = ind

    def scratch(name, shape, dt=F32):
        if dbg_in == name:
            return nc.dram_tensor(name, shape, dt, kind="ExternalInput").ap()
        if dbg_out == name:
            return nc.dram_tensor(name, shape, dt, kind="ExternalOutput").ap()
        return nc.dram_tensor(name, shape, dt).ap()

    C.x1_d = scratch("x1", [SEQ, D])
    C.x2_d = scratch("x2", [SEQ, D])
    C.x3_d = scratch("x3", [SEQ, D])
    C.yacc_d = scratch("yacc", [SEQ, D])
    C.hT_d = [nc.dram_tensor("hT%d" % i, [16, 128, NFC, 256], BF16).ap() for i in range(2)]
    C.o_d = nc.dram_tensor("o_d", [SEQ, D], BF16).ap()
    C.oT_d = nc.dram_tensor("oT_d", [D, SEQ], BF16).ap()
    C.lrow_d = nc.dram_tensor("lrow_d", [8, 2, 6, SEQ], BF16).ap()
    alloc_common(C)

    if "attn" in stages:
        stage_load_xT(C, ind["x"], "x")
        with S.scope():
            stage_attn(C)
        with S.scope():
            C.oT = S.sbuf("oT", [128, KC, SEQ], BF16)
            for kc in range(KC):
                S.dma([("oT_d", kc)], [("oTall",)], C.oT[:, kc, :], C.oT_d[kc * 128:(kc + 1) * 128, :])
            stage_outproj_ln(C, ind["attn_w_out"], ind["x"], "x", ind["ln_attn_g"], ind["ln_attn_b"], C.x1_d, "x1")
    if "ffn" in stages:
        if "attn" not in stages:
            stage_load_xT(C, C.x1_d, "x1")
        stage_ffn(C, [(ind["ffn_w_gate"], ind["ffn_w_up"], ind["ffn_w_down"])], C.x1_d, "x1",
                  ind["ln_ffn_g"], ind["ln_ffn_b"], C.x2_d, "x2")
    if "gdn" in stages:
        if "ffn" not in stages:
            stage_load_xT(C, C.x2_d, "x2")
        with S.scope():
            stage_gdn(C)
        with S.scope():
            C.oT = S.sbuf("oT", [128, KC, SEQ], BF16)
            stage_load_oT(C)
            stage_outproj_ln(C, ind["gdn_w_out"], C.x2_d, "x2", ind["ln_gdn_g"], ind["ln_gdn_b"], C.x3_d, "x3",
                             router=router_tile)
    if "moe" in stages:
        if "gdn" not in stages:
            with S.scope():
                alloc_ln(C)
                alloc_router(C)
                for t in range(NT):
                    i = t % 2
                    S.dma([("x3", t)], [("lnx", i)], C.lnx[i][:], C.x3_d[t * 128:(t + 1) * 128, :])
                    emit_xT(C, t, (C.lnx[i][:], ("lnx", i)))
                    router_tile(C, t, C.lnx[i], ("lnx", i))
        mg, mu, md = ind["moe_w_gate"], ind["moe_w_up"], ind["moe_w_down"]
        stage_ffn(C, [(mg[e], mu[e], md[e]) for e in range(NE)], C.x3_d, "x3",
                  ind["ln_moe_g"], ind["ln_moe_b"], out_d, "out", gate_sb=C.gate, make_xT=False)
    S.barrier()
    fin = {"ffn": C.x2_d, "attn": C.x1_d, "gdn": C.x3_d, "moe": None}[stages[-1]] if dbg_out is None else None
    if fin is not None:
        with S.scope():
            buf = [S.sbuf("fin", [128, D], F32) for _ in range(2)]
            for t in range(NT):
                i = t % 2
                S.dma([], [("fin", i)], buf[i][:], fin[t * 128:(t + 1) * 128, :])
                S.dma([("fin", i)], [("out", t)], out_d[t * 128:(t + 1) * 128, :], buf[i][:])
    S.barrier()
    print("n_inst", S.n_inst, S.cnt)
    S.close()
    return nc


def kernel(**inputs):
    nc = build(["attn", "ffn", "gdn", "moe"])
    consts = make_consts()
    shared = {}
    for k, shp in IN_SHAPES.items():
        if k == "x":
            continue
        shared[k] = np.ascontiguousarray(np.asarray(inputs[k], dtype=np.float32).reshape(shp))
    for k, v in consts.items():
        shared["c_" + k] = v
    x = np.asarray(inputs["x"], dtype=np.float32)
    in_maps = []
    for b in range(8):
        m = dict(shared)
        m["x"] = np.ascontiguousarray(x[b])
        in_maps.append(m)
    res = run_bass_kernel_spmd(nc, in_maps, core_ids=list(range(8)))
    return np.stack([np.asarray(r["out"], dtype=np.float32) for r in res.results], axis=0)
```
